# Optimizing a Trainium2 kernel written in Bass

```python
import math
import jax, jax.numpy as jnp
from jax import lax
import numpy as np

D_MODEL = 2048
BATCH = 16
SEQ = 2048
DEPTH = 1

D_MIX = D_MODEL
A_HEADS = 8
A_QK_DIM = 64
A_V_DIM = 2 * A_QK_DIM
A_WIDTH = A_HEADS * A_V_DIM
B_HEADS = 8
B_LAT = 256
B_V_DIM = 128
B_WIDTH = B_HEADS * B_V_DIM
IDX_HEADS = 16
IDX_DIM = 64
TOPK_MAX = 256
N_BUCKETS = 32
MAX_DISTANCE = 128
N_BIAS_HEADS = A_HEADS + B_HEADS
Q_BLOCK = 128
EPS = 1e-6

IN_SIZES = (
    2 * A_HEADS * A_QK_DIM,
    2 * A_HEADS * A_QK_DIM,
    A_HEADS * A_V_DIM,
    A_WIDTH,
    B_HEADS * B_LAT,
    B_LAT,
    B_WIDTH,
    IDX_HEADS * IDX_DIM,
    IDX_DIM,
    IDX_HEADS,
)
N_IN = 2 * A_HEADS * A_QK_DIM * 2 + A_HEADS * A_V_DIM + A_WIDTH + B_HEADS * B_LAT + B_LAT + B_WIDTH + IDX_HEADS * IDX_DIM + IDX_DIM + IDX_HEADS

kernel_name = "hymba_diff_dsa_hybrid_layer"


def _split_points():
    pts, acc = [], 0
    for s in IN_SIZES[:-1]:
        acc += s
        pts.append(acc)
    return pts


def rmsnorm(x, g):
    xf = x.astype(jnp.float32)
    y = xf * lax.rsqrt(jnp.mean(xf * xf, axis=-1, keepdims=True) + EPS)
    return (y * g.astype(jnp.float32)).astype(x.dtype)


def t5_bucket(dist):
    n = jnp.maximum(dist, 0)
    max_exact = N_BUCKETS // 2
    nf = jnp.maximum(n, 1).astype(jnp.float32)
    large = max_exact + (jnp.log(nf / max_exact) / math.log(MAX_DISTANCE / max_exact)
                         * (N_BUCKETS - max_exact)).astype(jnp.int32)
    large = jnp.minimum(large, N_BUCKETS - 1)
    return jnp.where(n < max_exact, n, large)


def diff_attention(q, k, v, bias_tab, lam, sub_g, lam_init):
    B, T = q.shape[0], q.shape[1]
    scale = A_QK_DIM ** -0.5
    pos = jnp.arange(T)
    neg = jnp.finfo(jnp.float32).min

    def block(i):
        q0 = i * Q_BLOCK
        qb = lax.dynamic_slice_in_dim(q, q0, Q_BLOCK, axis=1)
        tq = q0 + jnp.arange(Q_BLOCK)
        dist = tq[:, None] - pos[None, :]
        bias = jnp.transpose(bias_tab[t5_bucket(dist)], (2, 0, 1))
        logits = jnp.einsum('btchd,bschd->bchts', qb, k).astype(jnp.float32) * scale + bias
        logits = jnp.where(dist >= 0, logits, neg)
        p = jax.nn.softmax(logits, axis=-1)
        a = p[:, 0] - lam * p[:, 1]
        return jnp.einsum('bhts,bshd->bthd', a.astype(v.dtype), v)

    o = lax.map(block, jnp.arange(T // Q_BLOCK))
    o = jnp.transpose(o, (1, 0, 2, 3, 4)).reshape(B, T, A_HEADS, A_V_DIM)
    return rmsnorm(o, sub_g) * (1.0 - lam_init)


def dsa_attention(q, ckv, iq, ik, iw, bias_tab, w_uv):
    B, T = q.shape[0], q.shape[1]
    S = ckv.shape[1]
    topk = min(TOPK_MAX, S // 4)
    scale = B_LAT ** -0.5
    idx_scale = IDX_DIM ** -0.5
    head_w_scale = IDX_HEADS ** -0.5
    pos = jnp.arange(S)
    neg = jnp.finfo(jnp.float32).min
    gather = jax.vmap(lambda c, s: c[s])

    def block(i):
        q0 = i * Q_BLOCK
        qb = lax.dynamic_slice_in_dim(q, q0, Q_BLOCK, axis=1)
        iqb = lax.dynamic_slice_in_dim(iq, q0, Q_BLOCK, axis=1)
        iwb = lax.dynamic_slice_in_dim(iw, q0, Q_BLOCK, axis=1)
        tq = q0 + jnp.arange(Q_BLOCK)
        rel = jax.nn.relu(jnp.einsum('bthd,bsd->bths', iqb, ik).astype(jnp.float32) * idx_scale)
        score = jnp.einsum('bths,bth->bts', rel, iwb.astype(jnp.float32) * head_w_scale)
        score = jnp.where(pos[None, None, :] <= tq[None, :, None], score, neg)
        _, sel = lax.top_k(score, topk)
        valid = sel <= tq[None, :, None]
        kv_sel = gather(ckv, sel)
        bias = bias_tab[t5_bucket(tq[None, :, None] - sel)]
        logits = (jnp.einsum('bthc,btkc->bhtk', qb, kv_sel).astype(jnp.float32) * scale
                  + jnp.transpose(bias, (0, 3, 1, 2)))
        logits = jnp.where(valid[:, None], logits, neg)
        p = jax.nn.softmax(logits, axis=-1)
        o_lat = jnp.einsum('bhtk,btkc->bthc', p.astype(kv_sel.dtype), kv_sel)
        return jnp.einsum('bthc,hcd->bthd', o_lat, w_uv)

    o = lax.map(block, jnp.arange(T // Q_BLOCK))
    return jnp.transpose(o, (1, 0, 2, 3, 4)).reshape(B, T, B_HEADS, B_V_DIM)


def setup_inputs(seed: int = 0) -> dict:
    key = jax.random.key(seed)
    ks = jax.random.split(key, 16)
    f32 = jnp.float32
    nrm = lambda k, shape, s: (jax.random.normal(k, shape, f32) * s)
    return {
        "x": nrm(ks[0], (BATCH, SEQ, D_MODEL), 1.0),
        "norm_pre_g": 1.0 + nrm(ks[1], (DEPTH, D_MODEL), 0.02),
        "w_in": nrm(ks[2], (DEPTH, D_MODEL, N_IN), D_MODEL ** -0.5),
        "lambda_q1": nrm(ks[3], (DEPTH, A_QK_DIM), 0.1),
        "lambda_k1": nrm(ks[4], (DEPTH, A_QK_DIM), 0.1),
        "lambda_q2": nrm(ks[5], (DEPTH, A_QK_DIM), 0.1),
        "lambda_k2": nrm(ks[6], (DEPTH, A_QK_DIM), 0.1),
        "subln_g": 1.0 + nrm(ks[7], (DEPTH, A_V_DIM), 0.02),
        "kv_norm_g": 1.0 + nrm(ks[8], (DEPTH, B_LAT), 0.02),
        "idx_k_norm_g": 1.0 + nrm(ks[9], (DEPTH, IDX_DIM), 0.02),
        "w_uv": nrm(ks[10], (DEPTH, B_HEADS, B_LAT, B_V_DIM), B_LAT ** -0.5),
        "rel_bias": nrm(ks[11], (N_BUCKETS, N_BIAS_HEADS), 0.1),
        "w_out": nrm(ks[12], (DEPTH, D_MIX, D_MODEL), D_MIX ** -0.5),
        "norm_post_g": 1.0 + nrm(ks[13], (DEPTH, D_MODEL), 0.02),
    }


def reference(x, norm_pre_g, w_in, lambda_q1, lambda_k1, lambda_q2, lambda_k2, subln_g,
              kv_norm_g, idx_k_norm_g, w_uv, rel_bias, w_out, norm_post_g):
    B, T, _ = x.shape
    pts = _split_points()
    bias_a = rel_bias[:, :A_HEADS]
    bias_b = rel_bias[:, A_HEADS:]
    for l in range(DEPTH):
        lam_init = 0.8 - 0.6 * math.exp(-0.3 * l)
        h = rmsnorm(x, norm_pre_g[l])
        proj = jnp.einsum('btd,dn->btn', h, w_in[l])
        qa, ka, va, za, qb, ckv, zb, iq, ik, iw = jnp.split(proj, pts, axis=-1)

        lam = (jnp.exp(jnp.sum(lambda_q1[l].astype(jnp.float32) * lambda_k1[l].astype(jnp.float32)))
               - jnp.exp(jnp.sum(lambda_q2[l].astype(jnp.float32) * lambda_k2[l].astype(jnp.float32)))
               + lam_init)
        o_a = diff_attention(qa.reshape(B, T, 2, A_HEADS, A_QK_DIM),
                             ka.reshape(B, T, 2, A_HEADS, A_QK_DIM),
                             va.reshape(B, T, A_HEADS, A_V_DIM),
                             bias_a, lam, subln_g[l], lam_init)
        o_a = o_a.reshape(B, T, A_WIDTH) * jax.nn.silu(za)

        o_b = dsa_attention(qb.reshape(B, T, B_HEADS, B_LAT),
                            rmsnorm(ckv, kv_norm_g[l]),
                            iq.reshape(B, T, IDX_HEADS, IDX_DIM),
                            rmsnorm(ik, idx_k_norm_g[l]),
                            iw, bias_b, w_uv[l])
        o_b = o_b.reshape(B, T, B_WIDTH) * jax.nn.silu(zb)

        y = jnp.einsum('btm,md->btd', jnp.concatenate([o_a, o_b], axis=-1), w_out[l])
        x = x + rmsnorm(y, norm_post_g[l])
    return x
```

```python
import math
from contextlib import ExitStack

import numpy as np
import ml_dtypes

import concourse.bass as bass
import concourse.mybir as mybir
from concourse.bass_utils import run_bass_kernel_spmd

F32 = mybir.dt.float32
BF16 = mybir.dt.bfloat16
AF = mybir.ActivationFunctionType
ALU = mybir.AluOpType
AX = mybir.AxisListType

D = 2048
N_IN = 8528
EPS = 1e-6
NEG = -30000.0
NBIS = 14
BIS_W = 256.0
import os as _os
DEFER3 = int(_os.environ.get('K_DEFER3', '4'))
DEFER2 = int(_os.environ.get('K_DEFER2', '1'))


class Buf:
    __slots__ = ("w", "r", "name", "dsem", "excl")

    def __init__(self, name="", dsem=None, excl=False):
        self.w = {}
        self.r = {}
        self.name = name
        self.dsem = dsem
        self.excl = excl


class KB:
    def __init__(self, nc, es):
        self.nc = nc
        self.es = es
        self.E = {"pe": nc.tensor, "act": nc.scalar, "dve": nc.vector, "pool": nc.gpsimd, "sp": nc.sync}
        self.sem = {}
        self.cnt = {}
        self.seen = {e: {} for e in self.E}
        for e in self.E:
            self.sem[e] = es.enter_context(nc.semaphore("s_" + e))
            self.cnt[e] = 0
        self.nd = 0
        self.nwaits = 0

    def new_dsem(self):
        key = "d%d" % self.nd
        self.nd += 1
        self.sem[key] = self.es.enter_context(self.nc.semaphore(key))
        self.cnt[key] = 0
        return key

    def buf(self, name="", dma=False):
        return Buf(name, self.new_dsem() if dma else None)

    def _wait(self, e, key, val):
        if self.seen[e].get(key, 0) >= val:
            return
        self.E[e].wait_ge(self.sem[key], val)
        self.seen[e][key] = val
        self.nwaits += 1

    def begin(self, e, reads=(), writes=(), partial=False):
        deps = {}

        def add(k, v):
            if k == e and e == "pe":
                return
            if deps.get(k, 0) < v:
                deps[k] = v

        for b in reads:
            for k, v in b.w.items():
                add(k, v)
            if b.excl:
                for k, v in b.r.items():
                    if k != e:
                        add(k, v)
        for b in writes:
            for k, v in b.w.items():
                if not (k == e and partial):
                    add(k, v)
            for k, v in b.r.items():
                add(k, v)
        for k, v in deps.items():
            self._wait(e, k, v)

    def _mark(self, k, v, reads, writes, partial):
        for b in reads:
            if b.r.get(k, 0) < v:
                b.r[k] = v
        for b in writes:
            if partial:
                if b.w.get(k, 0) < v:
                    b.w[k] = v
            else:
                b.w = {k: v}
                b.r = {}

    def end(self, e, inst, reads=(), writes=(), partial=False):
        self.cnt[e] += 1
        inst.then_inc(self.sem[e], 1)
        self._mark(e, self.cnt[e], reads, writes, partial)

    def op(self, e, fn, reads=(), writes=(), partial=False):
        self.begin(e, reads, writes, partial)
        inst = fn()
        self.end(e, inst, reads, writes, partial)

    def dma(self, q, out, in_, dsem, reads=(), writes=(), partial=False):
        self.begin(q, reads, writes, partial)
        inst = self.E[q].dma_start(out=out, in_=in_)
        self.cnt[dsem] += 16
        inst.then_inc(self.sem[dsem], 16)
        self._mark(dsem, self.cnt[dsem], reads, writes, partial)

    def barrier(self):
        for e in self.E:
            for k in self.sem:
                if k != e and self.cnt[k] > 0:
                    self._wait(e, k, self.cnt[k])


class Ring:
    uid = 0

    def __init__(self, kb, es, nc, name, shape, dtype, n, dma=False):
        Ring.uid += 1
        self.t = [es.enter_context(nc.sbuf_tensor("%s%d_r%d" % (name, i, Ring.uid), shape, dtype)) for i in range(n)]
        self.b = [kb.buf("%s%d" % (name, i), dma) for i in range(n)]
        self.i = -1
        self.n = n

    def next(self):
        self.i = (self.i + 1) % self.n
        return self.t[self.i], self.b[self.i]


class CRing:
    def __init__(self, kb, es, nc, name, ncols, n):
        Ring.uid += 1
        self.t = [es.enter_context(nc.sbuf_tensor("%s%d_c%d" % (name, i, Ring.uid), [128, ncols], F32)) for i in range(n)]
        self.b = [[kb.buf("%s%d_%d" % (name, i, c)) for c in range(ncols)] for i in range(n)]
        self.i = -1
        self.n = n

    def next(self):
        self.i = (self.i + 1) % self.n
        return self.t[self.i], self.b[self.i]


def _t5_bucket_np(n):
    n = np.maximum(n, 0)
    nf = np.maximum(n, 1).astype(np.float32)
    large = 16 + (np.log(nf / np.float32(16)) / np.float32(math.log(128 / 16)) * np.float32(16)).astype(np.int32)
    large = np.minimum(large, 31)
    return np.where(n < 16, n, large)


def _consts():
    ident = np.eye(128, dtype=np.float32)
    jmat = ident[::-1].copy()
    oh = np.zeros((33, 510), np.float32)
    for case, delta in enumerate((0, 128)):
        for u in range(255):
            n = delta - 127 + u
            if n < 0:
                oh[32, case * 255 + u] = 1.0
            else:
                oh[int(_t5_bucket_np(np.array([n]))[0]), case * 255 + u] = 1.0
    negtri = np.where(np.arange(128)[None, :] > np.arange(128)[:, None], -1e30, 0.0).astype(np.float32)
    return {
        "ident_bf": ident.astype(ml_dtypes.bfloat16),
        "jmat_bf": jmat.astype(ml_dtypes.bfloat16),
        "oh": oh,
        "negtri_bf": negtri.astype(ml_dtypes.bfloat16),
    }


def build_nc(T=2048, NSEQ=2, dbg=False):
    NT = T // 128
    NTC = T // 512
    TOPK = min(256, T // 4)
    nc = bass.Bass("TRN2", target_bir_lowering=False)

    def din(name, shape, dt=F32):
        return nc.dram_tensor(name, list(shape), dt, kind="ExternalInput").ap()

    x_d = din("x", [NSEQ, T, D])
    win_d = din("w_in", [D, N_IN])
    wout_d = din("w_out", [D, D])
    gpre_d = din("norm_pre_g", [1, D])
    gpost_d = din("norm_post_g", [1, D])
    lq1_d = din("lambda_q1", [1, 64])
    lk1_d = din("lambda_k1", [1, 64])
    lq2_d = din("lambda_q2", [1, 64])
    lk2_d = din("lambda_k2", [1, 64])
    gsub_d = din("subln_g", [1, 128])
    gkv_d = din("kv_norm_g", [1, 256])
    gik_d = din("idx_k_norm_g", [1, 64])
    wuv_d = din("w_uv", [8, 256, 128])
    relb_d = din("rel_bias", [32, 16])
    ident_d = din("ident_bf", [128, 128], BF16)
    jmat_d = din("jmat_bf", [128, 128], BF16)
    oh_d = din("oh", [33, 510])
    negtri_d = din("negtri_bf", [128, 128], BF16)
    out_d = nc.dram_tensor("out", [NSEQ, T, D], F32, kind="ExternalOutput").ap()

    skind = "ExternalOutput" if dbg else "Internal"

    def dscr(name, shape, dt=BF16):
        return nc.dram_tensor(name, list(shape), dt, kind=skind).ap()

    QAT = dscr("s_qat", [NSEQ, 8, 128, T])
    KAT = dscr("s_kat", [NSEQ, 8, 128, T])
    VA = dscr("s_va", [NSEQ, T, 1024])
    GA = dscr("s_ga", [NSEQ, T, 1024])
    QBT = dscr("s_qbt", [NSEQ, 16, 128, T])
    GB = dscr("s_gb", [NSEQ, T, 1024])
    IQT = dscr("s_iqt", [NSEQ, 8, 128, T])
    GD = dscr("s_gd", [16, 510])
    if dbg:
        DBG_CKVT = dscr("s_ckvt", [NSEQ, 128, 2, T])
        DBG_IKT = dscr("s_ikt", [NSEQ, 128, 2, T])
        DBG_OT = dscr("s_ot", [NSEQ, 128, 16, T])
        DBG_THR = dscr("s_thr", [NSEQ, NT, 128, 1], F32)
        DBG_SC = dscr("s_sc", [NSEQ, NT, 128, T], F32)
        DBG_BS = dscr("s_bs", [NSEQ, NT, 128, 8], F32)
        DBG_Z8 = dscr("s_z8", [NSEQ, NT, 128, 24], F32)

    es = ExitStack()
    with es:
        kb = KB(nc, es)
        E = kb.E
        pe, act, dve, pool, sp = E["pe"], E["act"], E["dve"], E["pool"], E["sp"]

        uid = [0]

        def sb(name, shape, dt, stack=es):
            uid[0] += 1
            return stack.enter_context(nc.sbuf_tensor("%s_u%d" % (name, uid[0]), list(shape), dt))

        PS = es.enter_context(nc.psum_tensor("ps", [128, 8, 512], F32))
        PSB = [Buf("ps%d" % i, excl=True) for i in range(8)]
        bank_rr = [0]

        def nextbank(lo=0, hi=8):
            b = lo + bank_rr[0] % (hi - lo)
            bank_rr[0] += 1
            return b

        ident = sb("ident", [128, 128], BF16)
        jmat = sb("jmat", [128, 128], BF16)
        negtri = sb("negtri", [128, 128], BF16)
        oh_sb = sb("oh_sb", [33, 510], F32)
        cB = kb.buf("consts", dma=True)
        kb.dma("sp", ident[:], ident_d[:, :], cB.dsem, writes=[cB], partial=True)
        kb.dma("sp", jmat[:], jmat_d[:, :], cB.dsem, writes=[cB], partial=True)
        kb.dma("sp", negtri[:], negtri_d[:, :], cB.dsem, writes=[cB], partial=True)
        kb.dma("sp", oh_sb[:], oh_d[:, :], cB.dsem, writes=[cB], partial=True)

        def bcast(src2d, n):
            return bass.AP(src2d.tensor, src2d.offset, [[0, 128], [1, n]])

        g_bc = sb("g_bc", [128, D], F32)
        gB = kb.buf("g_bc", dma=True)
        gsub_bc = sb("gsub_bc", [128, 128], F32)
        gkv_bc = sb("gkv_bc", [128, 256], F32)
        gik_bc = sb("gik_bc", [128, 64], F32)
        lamt = sb("lamt", [128, 4, 64], F32)
        kb.dma("sp", gsub_bc[:], bcast(gsub_d[0:1, :], 128), cB.dsem, writes=[cB], partial=True)
        kb.dma("sp", gkv_bc[:], bcast(gkv_d[0:1, :], 256), cB.dsem, writes=[cB], partial=True)
        kb.dma("sp", gik_bc[:], bcast(gik_d[0:1, :], 64), cB.dsem, writes=[cB], partial=True)
        for i, l_d in enumerate((lq1_d, lk1_d, lq2_d, lk2_d)):
            kb.dma("sp", lamt[:, i, :], bcast(l_d[0:1, :], 64), cB.dsem, writes=[cB], partial=True)
        wuv = sb("wuv", [128, 8, 2, 128], BF16)
        wuvB = kb.buf("wuv", dma=True)
        kb.dma("pool", wuv[:], wuv_d.rearrange("h (cc p) n -> p h cc n", p=128), wuvB.dsem, writes=[wuvB])

        mhalf = sb("mhalf", [128, 1], F32)
        neg_lam = sb("neg_lam", [128, 1], F32)
        smallB = kb.buf("small")
        kb.op("dve", lambda: dve.memset(mhalf[:], -0.5), writes=[smallB], partial=True)
        iota8 = sb("iota8", [128, 8], F32)
        for c8 in range(8):
            kb.op("dve", lambda: dve.memset(iota8[:, c8:c8 + 1], float(c8)), writes=[smallB], partial=True)
        lprod = sb("lprod", [128, 2, 64], F32)
        lsum = sb("lsum", [128, 2], F32)
        lexp = sb("lexp", [128, 2], F32)
        lB = kb.buf("lamb")
        kb.op("dve", lambda: dve.tensor_tensor(out=lprod[:, 0, :], in0=lamt[:, 0, :], in1=lamt[:, 1, :], op=ALU.mult),
              reads=[cB], writes=[lB], partial=True)
        kb.op("dve", lambda: dve.tensor_tensor(out=lprod[:, 1, :], in0=lamt[:, 2, :], in1=lamt[:, 3, :], op=ALU.mult),
              reads=[cB], writes=[lB], partial=True)
        lB2 = kb.buf("lamb2")
        kb.op("dve", lambda: dve.tensor_reduce(out=lsum[:], in_=lprod[:], axis=AX.X, op=ALU.add), reads=[lB], writes=[lB2])
        lB3 = kb.buf("lamb3")
        kb.op("act", lambda: act.activation(out=lexp[:], in_=lsum[:], func=AF.Exp), reads=[lB2], writes=[lB3])
        lB4 = kb.buf("lamb4")
        ltmp = sb("ltmp", [128, 1], F32)
        kb.op("dve", lambda: dve.tensor_tensor(out=ltmp[:], in0=lexp[:, 1:2], in1=lexp[:, 0:1], op=ALU.subtract),
              reads=[lB3], writes=[lB4])
        kb.op("dve", lambda: dve.tensor_scalar(out=neg_lam[:], in0=ltmp[:], scalar1=-0.2, scalar2=None, op0=ALU.add),
              reads=[lB4], writes=[smallB], partial=True)
        gsB = kb.buf("gsub")
        kb.op("dve", lambda: dve.tensor_scalar(out=gsub_bc[:], in0=gsub_bc[:], scalar1=0.8, scalar2=None, op0=ALU.mult),
              reads=[cB], writes=[gsB])

        tab33 = sb("tab33", [33, 16], F32)
        r31 = sb("r31", [32, 16], F32)
        tB = kb.buf("tab", dma=True)
        kb.dma("sp", tab33[0:32, :], relb_d[:, :], tB.dsem, writes=[tB], partial=True)
        kb.dma("sp", r31[:], bass.AP(relb_d.tensor, relb_d[31:32, :].offset, [[0, 32], [1, 16]]), tB.dsem,
               writes=[tB], partial=True)
        tB2 = kb.buf("tab2")
        kb.op("dve", lambda: dve.tensor_tensor(out=tab33[0:32, :], in0=tab33[0:32, :], in1=r31[:], op=ALU.subtract),
              reads=[tB], writes=[tB2], partial=True)
        kb.op("dve", lambda: dve.memset(tab33[32:33, :], NEG), writes=[tB2], partial=True)
        gd_sb = sb("gd_sb", [16, 510], BF16)
        gdB = kb.buf("gd", dma=True)
        kb.begin("pe", reads=[tB2, cB], writes=[PSB[0]])
        mm = pe.matmul(PS[0:16, 0, 0:510], tab33[:, :], oh_sb[:, :], start=True, stop=True)
        kb.end("pe", mm, reads=[tB2, cB], writes=[PSB[0]])
        kb.op("act", lambda: act.activation(out=gd_sb[:], in_=PS[0:16, 0, 0:510], func=AF.Copy), reads=[PSB[0]], writes=[gdB])
        GDB = kb.buf("GD", dma=True)
        kb.dma("sp", GD[:, :], gd_sb[:], gdB.dsem, reads=[gdB], writes=[GDB])
        XAB = sb("xab", [128, 16, 2, 128], BF16)
        xabB = kb.buf("xab", dma=True)
        for case in range(2):
            kb.dma("sp", XAB[:, :, case, :],
                   bass.AP(GD.tensor, GD.offset + case * 255, [[1, 128], [510, 16], [1, 128]]),
                   xabB.dsem, reads=[GDB], writes=[xabB], partial=True)

        ckvT = sb("ckvT", [128, 2, T], BF16)
        ikT2 = sb("ikT2", [128, 2, T], BF16)
        iw_sb = sb("iw_sb", [128, NT, 16], F32)
        ckvTB = kb.buf("ckvT")
        ikTB = kb.buf("ikT")
        iwB = kb.buf("iw")
        oTB = kb.buf("oT")

        def rstd_ops(ss_ap, ssB, out_t, outB, inv_n, tmp_t, tmpB):
            kb.op("act", lambda: act.activation(out=tmp_t, in_=ss_ap, func=AF.Ln, scale=inv_n, bias=EPS),
                  reads=[ssB], writes=[tmpB])
            kb.op("act", lambda: act.activation(out=out_t, in_=tmp_t, func=AF.Exp, scale=-0.5),
                  reads=[tmpB], writes=[outB])

        for b in range(NSEQ):
            with ExitStack() as p1:
                hT = sb("hT", [128, 16, T], BF16, p1)
                hTB = [kb.buf("hT%d" % i) for i in range(NTC)]
                xt_r = Ring(kb, p1, nc, "xt", [128, D], F32, 2, dma=True)
                hb_r = Ring(kb, p1, nc, "hb", [128, D], BF16, 2)
                junk = sb("junk1", [128, D], BF16, p1)
                junkB = kb.buf("junk")
                st_r = CRing(kb, p1, nc, "st1", 6, 4)
                wt_r = Ring(kb, p1, nc, "wt", [128, 16, 512], BF16, 3, dma=True)
                fst_r = Ring(kb, p1, nc, "fst", [128, T], BF16, 2, dma=True)
                tst_r = Ring(kb, p1, nc, "tst", [128, 512], BF16, 3, dma=True)
                ef_r = Ring(kb, p1, nc, "ef", [128, 512], F32, 2)
                ckn_r = Ring(kb, p1, nc, "ckn", [128, 512], BF16, 2)
                for t_, tB_ in zip(ckn_r.t, ckn_r.b):
                    kb.op("dve", lambda: dve.memset(t_[:, 320:448], 0.0), writes=[tB_], partial=True)
                scrB = [kb.buf("scr%d" % i, dma=True) for i in range(8)]

                kb.dma("sp", g_bc[:], bcast(gpre_d[0:1, :], D), gB.dsem, writes=[gB])
                def stage_a(tb):
                    xt, xtB = xt_r.next()
                    kb.dma("sp", xt[:], x_d[b, tb * 128:(tb + 1) * 128, :], xtB.dsem, writes=[xtB])
                    st, sB_ = st_r.next()
                    ssB, vB, rB = sB_[0], sB_[1], sB_[2]
                    kb.op("act", lambda: act.activation(out=junk[:], in_=xt[:], func=AF.Square, accum_out=st[:, 0:1]),
                          reads=[xtB], writes=[junkB, ssB])
                    rstd_ops(st[:, 0:1], ssB, st[:, 2:3], rB, 1.0 / D, st[:, 1:2], vB)
                    hb, hbB = hb_r.next()
                    kb.op("dve", lambda: dve.scalar_tensor_tensor(out=hb[:], in0=xt[:], scalar=st[:, 2:3], in1=g_bc[:],
                                                                 op0=ALU.mult, op1=ALU.mult),
                          reads=[xtB, rB, gB], writes=[hbB])
                    return hb, hbB

                def stage_b(tb, hb, hbB):
                    for q in range(4):
                        bk = nextbank()
                        kb.begin("pe", reads=[hbB, cB], writes=[PSB[bk]])
                        for k4 in range(4):
                            k = q * 4 + k4
                            mm = pe.matmul(PS[:, bk, k4 * 128:(k4 + 1) * 128], hb[:, k * 128:(k + 1) * 128], ident[:, :],
                                           start=True, stop=True)
                        kb.end("pe", mm, reads=[hbB, cB], writes=[PSB[bk]])
                        src = PS[:, bk, :].rearrange("p (k n) -> p k n", k=4)
                        dst = hT[:, q * 4:(q + 1) * 4, tb * 128:(tb + 1) * 128]
                        if q % 2 == 0:
                            kb.op("act", lambda: act.activation(out=dst, in_=src, func=AF.Copy),
                                  reads=[PSB[bk]], writes=[hTB[tb // 4]], partial=True)
                        else:
                            kb.op("dve", lambda: dve.tensor_copy(out=dst, in_=src),
                                  reads=[PSB[bk]], writes=[hTB[tb // 4]], partial=True)

                prev_a = None
                for tb in range(NT):
                    cur = stage_a(tb)
                    if prev_a is not None:
                        stage_b(tb - 1, *prev_a)
                    prev_a = cur
                stage_b(NT - 1, *prev_a)

                def load_w(cols):
                    wt, wtB = wt_r.next()
                    off = 0
                    first = True
                    for (c0, w) in cols:
                        kb.dma("pool", wt[:, :, off:off + w],
                               win_d[:, c0:c0 + w].rearrange("(k p) n -> p k n", p=128),
                               wtB.dsem, writes=[wtB], partial=not first)
                        first = False
                        off += w
                    return wt, wtB

                def feat_group(wt, wtB, c0, dst, chunk0, scale, sB):
                    for m in range(4):
                        stg, stgB = fst_r.next()
                        for tc in range(NTC):
                            bk = nextbank()
                            kb.begin("pe", reads=[wtB, hTB[tc]], writes=[PSB[bk]])
                            for k in range(16):
                                mm = pe.matmul(PS[:, bk, :], wt[:, k, m * 128:(m + 1) * 128], hT[:, k, tc * 512:(tc + 1) * 512],
                                               start=(k == 0), stop=(k == 15))
                            kb.end("pe", mm, reads=[wtB, hTB[tc]], writes=[PSB[bk]])
                            o_ap = stg[:, tc * 512:(tc + 1) * 512]
                            i_ap = PS[:, bk, :]
                            kb.op("act", lambda: act.activation(out=o_ap, in_=i_ap, func=AF.Copy, scale=scale),
                                  reads=[PSB[bk]], writes=[stgB], partial=(tc > 0))
                        kb.dma("sp", dst[b, chunk0 + m, :, :], stg[:], stgB.dsem, reads=[stgB], writes=[sB], partial=True)

                def tok_group(wt, wtB, c0, dst, dcol0, gate, sB):
                    for tb in range(NT):
                        bk = nextbank()
                        kb.begin("pe", reads=[wtB, hTB[tb // 4]], writes=[PSB[bk]])
                        for k in range(16):
                            mm = pe.matmul(PS[:, bk, :], hT[:, k, tb * 128:(tb + 1) * 128], wt[:, k, :],
                                           start=(k == 0), stop=(k == 15))
                        kb.end("pe", mm, reads=[wtB, hTB[tb // 4]], writes=[PSB[bk]])
                        stg, stgB = tst_r.next()
                        if not gate:
                            kb.op("dve", lambda: dve.tensor_copy(out=stg[:], in_=PS[:, bk, :]), reads=[PSB[bk]], writes=[stgB])
                        else:
                            ef, efB = ef_r.next()
                            kb.op("act", lambda: act.activation(out=ef[:], in_=PS[:, bk, :], func=AF.Exp, scale=-1.0),
                                  reads=[PSB[bk]], writes=[efB])
                            kb.op("act", lambda: act.activation(out=ef[:], in_=ef[:], func=AF.Ln, bias=1.0), reads=[efB], writes=[efB])
                            kb.op("act", lambda: act.activation(out=ef[:], in_=ef[:], func=AF.Exp, scale=-1.0),
                                  reads=[efB], writes=[efB])
                            kb.op("dve", lambda: dve.tensor_tensor(out=stg[:], in0=PS[:, bk, :], in1=ef[:], op=ALU.mult),
                                  reads=[PSB[bk], efB], writes=[stgB])
                        kb.dma("sp", dst[b, tb * 128:(tb + 1) * 128, dcol0:dcol0 + 512], stg[:], stgB.dsem,
                               reads=[stgB], writes=[sB], partial=True)

                def lat_group(wt, wtB):
                    for tb in range(NT):
                        bk = nextbank()
                        kb.begin("pe", reads=[wtB, hTB[tb // 4]], writes=[PSB[bk]])
                        for k in range(16):
                            mm = pe.matmul(PS[:, bk, 0:336], hT[:, k, tb * 128:(tb + 1) * 128], wt[:, k, 0:336],
                                           start=(k == 0), stop=(k == 15))
                        kb.end("pe", mm, reads=[wtB, hTB[tb // 4]], writes=[PSB[bk]])
                        st, sB_ = st_r.next()
                        kb.op("act", lambda: act.activation(out=junk[:, 0:256], in_=PS[:, bk, 0:256], func=AF.Square,
                                                            accum_out=st[:, 0:1]),
                              reads=[PSB[bk]], writes=[junkB, sB_[0]])
                        kb.op("act", lambda: act.activation(out=junk[:, 256:320], in_=PS[:, bk, 256:320], func=AF.Square,
                                                            accum_out=st[:, 1:2]),
                              reads=[PSB[bk]], writes=[junkB, sB_[1]])
                        rstd_ops(st[:, 0:1], sB_[0], st[:, 4:5], sB_[4], 1.0 / 256, st[:, 2:3], sB_[2])
                        rstd_ops(st[:, 1:2], sB_[1], st[:, 5:6], sB_[5], 1.0 / 64, st[:, 3:4], sB_[3])
                        ckn, cknB = ckn_r.next()
                        kb.op("dve", lambda: dve.scalar_tensor_tensor(out=ckn[:, 0:256], in0=PS[:, bk, 0:256], scalar=st[:, 4:5],
                                                                     in1=gkv_bc[:], op0=ALU.mult, op1=ALU.mult),
                              reads=[PSB[bk], sB_[4], cB], writes=[cknB], partial=True)
                        for hh in range(2):
                            kb.op("dve", lambda: dve.scalar_tensor_tensor(out=ckn[:, 256 + hh * 192:320 + hh * 192],
                                                                         in0=PS[:, bk, 256:320], scalar=st[:, 5:6],
                                                                         in1=gik_bc[:], op0=ALU.mult, op1=ALU.mult),
                                  reads=[PSB[bk], sB_[5], cB], writes=[cknB], partial=True)
                        kb.op("dve", lambda: dve.tensor_copy(out=iw_sb[:, tb, :], in_=PS[:, bk, 320:336]),
                              reads=[PSB[bk]], writes=[iwB], partial=True)
                        bk2 = nextbank()
                        kb.begin("pe", reads=[cknB, cB], writes=[PSB[bk2]])
                        for j in range(4):
                            mm = pe.matmul(PS[:, bk2, j * 128:(j + 1) * 128], ckn[:, j * 128:(j + 1) * 128], ident[:, :],
                                           start=True, stop=True)
                        kb.end("pe", mm, reads=[cknB, cB], writes=[PSB[bk2]])
                        kb.op("act", lambda: act.activation(out=ckvT[:, :, tb * 128:(tb + 1) * 128],
                                                            in_=PS[:, bk2, 0:256].rearrange("p (c n) -> p c n", c=2), func=AF.Copy),
                              reads=[PSB[bk2]], writes=[ckvTB], partial=True)
                        kb.op("act", lambda: act.activation(out=ikT2[:, :, tb * 128:(tb + 1) * 128],
                                                            in_=PS[:, bk2, 256:512].rearrange("p (c n) -> p c n", c=2),
                                                            func=AF.Copy),
                              reads=[PSB[bk2]], writes=[ikTB], partial=True)

                specs = [
                    ("f", [(0, 512)], (0, QAT, 0, 0.125, scrB[0])),
                    ("f", [(512, 512)], (512, QAT, 4, 0.125, scrB[0])),
                    ("f", [(1024, 512)], (1024, KAT, 0, 1.0, scrB[1])),
                    ("f", [(1536, 512)], (1536, KAT, 4, 1.0, scrB[1])),
                    ("t", [(2048, 512)], (2048, VA, 0, False, scrB[2])),
                    ("t", [(2560, 512)], (2560, VA, 512, False, scrB[2])),
                    ("t", [(3072, 512)], (3072, GA, 0, True, scrB[3])),
                    ("t", [(3584, 512)], (3584, GA, 512, True, scrB[3])),
                ]
                for g in range(4):
                    specs.append(("f", [(4096 + g * 512, 512)], (4096 + g * 512, QBT, g * 4, 0.0625, scrB[4])))
                specs.append(("l", [(6144, 256), (8448, 80)], ()))
                specs += [
                    ("t", [(6400, 512)], (6400, GB, 0, True, scrB[5])),
                    ("t", [(6912, 512)], (6912, GB, 512, True, scrB[5])),
                    ("f", [(7424, 512)], (7424, IQT, 0, 1.0, scrB[6])),
                    ("f", [(7936, 512)], (7936, IQT, 4, 1.0, scrB[6])),
                ]
                loaded = {}
                for gi, (kind, cols, args) in enumerate(specs):
                    for a in range(gi, min(gi + 3, len(specs))):
                        if a not in loaded:
                            loaded[a] = load_w(specs[a][1])
                    wt, wtB = loaded.pop(gi)
                    if kind == "f":
                        feat_group(wt, wtB, *args)
                    elif kind == "t":
                        tok_group(wt, wtB, *args)
                    else:
                        lat_group(wt, wtB)
                if dbg:
                    dbgB = kb.buf("dbg", dma=True)
                    kb.dma("sp", DBG_CKVT[b], ckvT[:], dbgB.dsem, reads=[ckvTB], writes=[dbgB], partial=True)
                    kb.dma("sp", DBG_IKT[b], ikT2[:], dbgB.dsem, reads=[ikTB], writes=[dbgB], partial=True)
                kb.barrier()

            p234 = ExitStack()
            p234.__enter__()
            oT = sb("oT", [128, 16, T], BF16, p234)
            with ExitStack() as p2:
                qt_r = Ring(kb, p2, nc, "qt", [128, 2, T], BF16, 2, dma=True)
                kt_r = Ring(kb, p2, nc, "kt", [128, 2, 2, T], BF16, 2, dma=True)
                for t_, tB_ in zip(kt_r.t, kt_r.b):
                    kb.op("dve", lambda: dve.memset(t_[:], 0.0), writes=[tB_], partial=True)
                vt_r = Ring(kb, p2, nc, "vt", [128, NT, 2, 129], BF16, 2, dma=True)
                gt_r = Ring(kb, p2, nc, "gt", [128, NT, 256], BF16, 2, dma=True)
                pt_r = Ring(kb, p2, nc, "pt", [128, 512], BF16, 4)
                o1_r = Ring(kb, p2, nc, "o1", [128, 128], F32, 4)
                o2_r = Ring(kb, p2, nc, "o2", [128, 128], F32, 4)
                ot_r = Ring(kb, p2, nc, "ot", [128, 128], BF16, 4)
                sc_r = CRing(kb, p2, nc, "sc2", 8, 4)
                junk2 = sb("junk2", [128, 128], F32, p2)
                junk2B = kb.buf("junk2")
                for t_, tB_ in zip(vt_r.t, vt_r.b):
                    kb.op("dve", lambda: dve.memset(t_[:, :, :, 128:129], 1.0), writes=[tB_], partial=True)
                TB_ = 7
                trc = [0]
                defer2 = []
                tile_no = [0]
                for hp in range(4):
                    qt, qtB = qt_r.next()
                    kt, ktB = kt_r.next()
                    vt, vtB = vt_r.next()
                    gt, gtB = gt_r.next()
                    qv = QAT[b].rearrange("(c m) p t -> m p c t", c=2)
                    kv = KAT[b].rearrange("(c m) p t -> m p c t", c=2)
                    kb.dma("sp", qt[:], qv[hp], qtB.dsem, reads=[scrB[0]], writes=[qtB])
                    for hh_ in range(2):
                        kb.dma("sp", kt[hh_ * 64:(hh_ + 1) * 64, :, hh_, :], kv[hp][hh_ * 64:(hh_ + 1) * 64], ktB.dsem,
                               reads=[scrB[1]], writes=[ktB], partial=True)
                    vv = VA[b].rearrange("(j p) (h d) -> p j h d", p=128, d=128)
                    for hh_ in range(2):
                        kb.dma("sp", vt[:, :, hh_, 0:128], vv[:, :, 2 * hp + hh_, :], vtB.dsem, reads=[scrB[2]], writes=[vtB],
                               partial=True)
                    gv = GA[b].rearrange("(j p) c -> p j c", p=128)
                    kb.dma("sp", gt[:], gv[:, :, hp * 256:(hp + 1) * 256], gtB.dsem, reads=[scrB[3]], writes=[gtB])

                    for i in range(NT):
                        par = tile_no[0] % 2
                        tile_no[0] += 1
                        OB = [3 + 2 * par, 4 + 2 * par]
                        units = []
                        ng = (i + 4) // 4
                        for c in range(2):
                            for g in range(ng):
                                for hh in range(2):
                                    units.append((c, g, hh))
                        pend = []

                        def qk_exp(u, idx):
                            c, g, hh = u
                            js = list(range(4 * g, min(4 * g + 4, i + 1)))
                            bk = idx % 3
                            rd = [qtB, ktB, xabB, cB]
                            kb.begin("pe", reads=rd, writes=[PSB[bk]])
                            for jj, j in enumerate(js):
                                near = j >= i - 1
                                mm = pe.matmul(PS[:, bk, jj * 128:(jj + 1) * 128],
                                               kt[:, c, hh, j * 128:(j + 1) * 128],
                                               qt[:, c, i * 128:(i + 1) * 128],
                                               start=True, stop=not near)
                                if near:
                                    case = 0 if j == i else 1
                                    mm = pe.matmul(PS[:, bk, jj * 128:(jj + 1) * 128], jmat[:, :],
                                                   XAB[:, 2 * hp + hh, case, :], start=False, stop=True)
                            kb.end("pe", mm, reads=rd, writes=[PSB[bk]])
                            pt, ptB = pt_r.next()
                            w = len(js) * 128
                            kb.op("act", lambda: act.activation(out=pt[:, 0:w], in_=PS[:, bk, 0:w], func=AF.Exp),
                                  reads=[PSB[bk]], writes=[ptB])
                            return (u, js, pt, ptB)

                        def pv(item):
                            (c, g, hh), js, pt, ptB = item
                            ob = OB[hh]
                            kb.begin("pe", reads=[ptB, vtB], writes=[PSB[ob]])
                            for jj, j in enumerate(js):
                                mm = pe.matmul(PS[:, ob, c * 129:(c + 1) * 129], pt[:, jj * 128:(jj + 1) * 128],
                                               vt[:, j, hh, :], start=(j == 0), stop=(j == i))
                            kb.end("pe", mm, reads=[ptB, vtB], writes=[PSB[ob]], partial=not (c == 0 and g == 0))

                        for idx, u in enumerate(units):
                            pend.append(qk_exp(u, idx))
                            if len(pend) > 2:
                                pv(pend.pop(0))
                        while pend:
                            pv(pend.pop(0))

                        tmps = []
                        for hh in range(2):
                            sc, scB = sc_r.next()
                            o1, o1B = o1_r.next()
                            o2, o2B = o2_r.next()
                            ot, otB = ot_r.next()
                            tmps.append((sc, scB, o1, o1B, o2, o2B, ot, otB, scB[0], scB[2], scB[3], scB[4], scB[5]))
                        for hh in range(2):
                            sc, scB, o1, o1B, o2, o2B, ot, otB, recB, nlB, ssB, vB, rB = tmps[hh]
                            ob = OB[hh]
                            den = PS[:, ob, 0:258].rearrange("p (c n) -> p c n", c=2)[:, :, 128]
                            kb.op("dve", lambda: dve.reciprocal(out=sc[:, 0:2], in_=den), reads=[PSB[ob]], writes=[recB, scB[1]])
                        for hh in range(2):
                            sc, scB, o1, o1B, o2, o2B, ot, otB, recB, nlB, ssB, vB, rB = tmps[hh]
                            kb.op("dve", lambda: dve.tensor_tensor(out=sc[:, 2:3], in0=sc[:, 1:2], in1=neg_lam[:], op=ALU.mult),
                                  reads=[recB, smallB], writes=[nlB])
                        for hh in range(2):
                            sc, scB, o1, o1B, o2, o2B, ot, otB, recB, nlB, ssB, vB, rB = tmps[hh]
                            ob = OB[hh]
                            kb.op("dve", lambda: dve.tensor_scalar(out=o1[:], in0=PS[:, ob, 0:128], scalar1=sc[:, 0:1], scalar2=None,
                                                                   op0=ALU.mult), reads=[PSB[ob], recB], writes=[o1B])
                        for hh in range(2):
                            sc, scB, o1, o1B, o2, o2B, ot, otB, recB, nlB, ssB, vB, rB = tmps[hh]
                            ob = OB[hh]
                            kb.op("dve", lambda: dve.scalar_tensor_tensor(out=o2[:], in0=PS[:, ob, 129:257], scalar=sc[:, 2:3],
                                                                         in1=o1[:], op0=ALU.mult, op1=ALU.add),
                                  reads=[PSB[ob], nlB, o1B], writes=[o2B])
                        for hh in range(2):
                            sc, scB, o1, o1B, o2, o2B, ot, otB, recB, nlB, ssB, vB, rB = tmps[hh]
                            kb.op("dve", lambda: dve.scalar_tensor_tensor(out=junk2[:], in0=o2[:], scalar=1.0, in1=o2[:],
                                                                         op0=ALU.mult, op1=ALU.mult, accum_out=sc[:, 3:4]),
                                  reads=[o2B], writes=[junk2B, ssB])
                        for hh in range(2):
                            sc, scB, o1, o1B, o2, o2B, ot, otB, recB, nlB, ssB, vB, rB = tmps[hh]
                            rstd_ops(sc[:, 3:4], ssB, sc[:, 5:6], rB, 1.0 / 128, sc[:, 4:5], vB)
                        for hh in range(2):
                            sc, scB, o1, o1B, o2, o2B, ot, otB, recB, nlB, ssB, vB, rB = tmps[hh]
                            kb.op("dve", lambda: dve.scalar_tensor_tensor(out=o1[:], in0=o2[:], scalar=sc[:, 5:6], in1=gsub_bc[:],
                                                                         op0=ALU.mult, op1=ALU.mult),
                                  reads=[o2B, rB, gsB], writes=[o1B])
                        for hh in range(2):
                            sc, scB, o1, o1B, o2, o2B, ot, otB, recB, nlB, ssB, vB, rB = tmps[hh]
                            kb.op("dve", lambda: dve.tensor_tensor(out=ot[:], in0=o1[:], in1=gt[:, i, hh * 128:(hh + 1) * 128],
                                                                   op=ALU.mult), reads=[o1B, gtB], writes=[otB])
                        def tr_closure(tm=tmps, hp_=hp, i_=i):
                            tbk = 7
                            kb.begin("pe", reads=[tm[0][7], tm[1][7], cB], writes=[PSB[tbk]])
                            for hh_ in range(2):
                                mm_ = pe.matmul(PS[:, tbk, hh_ * 128:(hh_ + 1) * 128], tm[hh_][6][:, :], ident[:, :],
                                                start=True, stop=True)
                            kb.end("pe", mm_, reads=[tm[0][7], tm[1][7], cB], writes=[PSB[tbk]])
                            kb.op("act", lambda: act.activation(out=oT[:, 2 * hp_:2 * hp_ + 2, i_ * 128:(i_ + 1) * 128],
                                                                in_=PS[:, tbk, 0:256].rearrange("p (c n) -> p c n", c=2),
                                                                func=AF.Copy),
                                  reads=[PSB[tbk]], writes=[oTB], partial=True)
                        defer2.append(tr_closure)
                        while len(defer2) > DEFER2:
                            defer2.pop(0)()
                while defer2:
                    defer2.pop(0)()
                kb.barrier()

            with ExitStack() as p3:
                VB = sb("VB", [128, NT, 8, 129], BF16, p3)
                VBB = kb.buf("VB")
                qbt_r = Ring(kb, p3, nc, "qbt", [128, 16, 128], BF16, 2, dma=True)
                iqt_r = Ring(kb, p3, nc, "iqt", [128, 8, 128], BF16, 2, dma=True)
                gbt_r = Ring(kb, p3, nc, "gbt", [128, 1024], BF16, 2, dma=True)
                diag_r = Ring(kb, p3, nc, "diag", [128, 16, 128], BF16, 1)
                rr_r = Ring(kb, p3, nc, "rr", [128, 512], BF16, 5)
                pt_r = Ring(kb, p3, nc, "pt3", [128, 512], BF16, 5)
                score = sb("score", [128, T], F32, p3)
                scoreB = kb.buf("score")
                nm_r = Ring(kb, p3, nc, "nm", [128, T], BF16, 3)
                bs_r = CRing(kb, p3, nc, "bs", 8, 3)
                zbuf = sb("zbuf", [128, T], F32, p3)
                zB = kb.buf("zbuf")
                z8_r = Ring(kb, p3, nc, "z8", [128, 24], F32, 2)
                ot_r = Ring(kb, p3, nc, "ot3", [128, 128], BF16, 12)
                rc_r = Ring(kb, p3, nc, "rc3", [128, 1], F32, 4)
                deferred = []

                def run_deferred(keep):
                    while len(deferred) > keep:
                        n = min(4, len(deferred))
                        while n > 1 and not (deferred[n - 1][3] == deferred[0][3] and deferred[n - 1][2] == deferred[0][2] + n - 1):
                            n -= 1
                        batch = [deferred.pop(0) for _ in range(n)]
                        rdl = [x_[1] for x_ in batch] + [cB]
                        kb.begin("pe", reads=rdl, writes=[PSB[TB_]])
                        for q, (ot_, otB_, h_, i_) in enumerate(batch):
                            mm_ = pe.matmul(PS[:, TB_, q * 128:(q + 1) * 128], ot_[:, :], ident[:, :], start=True, stop=True)
                        kb.end("pe", mm_, reads=rdl, writes=[PSB[TB_]])
                        h0, i0 = batch[0][2], batch[0][3]
                        kb.op("act", lambda: act.activation(out=oT[:, 8 + h0:8 + h0 + n, i0 * 128:(i0 + 1) * 128],
                                                            in_=PS[:, TB_, 0:n * 128].rearrange("p (c n) -> p c n", c=n),
                                                            func=AF.Copy),
                              reads=[PSB[TB_]], writes=[oTB], partial=True)

                kb.op("dve", lambda: dve.memset(VB[:, :, :, 128:129], 1.0), writes=[VBB], partial=True)
                for j in range(NT):
                    for half in range(2):
                        bk = nextbank(0, 4)
                        kb.begin("pe", reads=[ckvTB, wuvB], writes=[PSB[bk]])
                        for h4 in range(4):
                            h = half * 4 + h4
                            for cc in range(2):
                                mm = pe.matmul(PS[:, bk, h4 * 128:(h4 + 1) * 128], ckvT[:, cc, j * 128:(j + 1) * 128],
                                               wuv[:, h, cc, :], start=(cc == 0), stop=(cc == 1))
                        kb.end("pe", mm, reads=[ckvTB, wuvB], writes=[PSB[bk]])
                        kb.op("act", lambda: act.activation(out=VB[:, j, half * 4:(half + 1) * 4, 0:128],
                                                            in_=PS[:, bk, :].rearrange("p (h n) -> p h n", h=4), func=AF.Copy),
                              reads=[PSB[bk]], writes=[VBB], partial=True)

                XBK = [0, 1]
                SBK = [2, 3]
                OB = [4, 5]
                SCB = 6
                TB_ = 7
                xcnt = [0]
                scnt = [0]
                nm_of = {}

                def gen_index(i):
                    L = (i + 1) * 128
                    iqt, iqtB = iqt_r.next()
                    kb.dma("sp", iqt[:], IQT[b].rearrange("c p t -> p c t")[:, :, i * 128:(i + 1) * 128], iqtB.dsem,
                           reads=[scrB[6]], writes=[iqtB])
                    diag, diagB = diag_r.next()
                    id_ap = ident[:, :]
                    iw_ap = iw_sb[:, i, :]
                    id_bc = bass.AP(id_ap.tensor, id_ap.offset, [list(id_ap.ap[0]), [0, 16], [1, 128]])
                    iw_bc = bass.AP(iw_ap.tensor, iw_ap.offset, [list(iw_ap.ap[0]), [1, 16], [0, 128]])
                    kb.op("dve", lambda: dve.tensor_tensor(out=diag[:], in0=id_bc, in1=iw_bc, op=ALU.mult),
                          reads=[cB, iwB], writes=[diagB])
                    yield
                    nsc = (L + 511) // 512
                    for sc_i in range(nsc):
                        s0 = sc_i * 512
                        w = min(512, L - s0)
                        pend = []

                        def idx_x(h16):
                            m, hh = h16 // 2, h16 % 2
                            bk = xcnt[0] % 4
                            xcnt[0] += 1
                            kb.begin("pe", reads=[iqtB, ikTB], writes=[PSB[bk]])
                            mm = pe.matmul(PS[:, bk, 0:w], iqt[:, m, :], ikT2[:, hh, s0:s0 + w], start=True, stop=True)
                            kb.end("pe", mm, reads=[iqtB, ikTB], writes=[PSB[bk]])
                            rr, rrB = rr_r.next()
                            if h16 % 4 == 0:
                                kb.op("act", lambda: act.activation(out=rr[:, 0:w], in_=PS[:, bk, 0:w], func=AF.Relu),
                                      reads=[PSB[bk]], writes=[rrB])
                            else:
                                kb.op("dve", lambda: dve.tensor_scalar(out=rr[:, 0:w], in0=PS[:, bk, 0:w], scalar1=0.0, scalar2=None,
                                                                       op0=ALU.max), reads=[PSB[bk]], writes=[rrB])
                            return (h16, rr, rrB)

                        def idx_acc(item):
                            h16, rr, rrB = item
                            last = (h16 == 15)
                            has_diag = (s0 + w == L)
                            kb.begin("pe", reads=[rrB, diagB, cB], writes=[PSB[SCB]])
                            mm = pe.matmul(PS[:, SCB, 0:w], diag[:, h16, :], rr[:, 0:w], start=(h16 == 0),
                                           stop=(last and not has_diag))
                            if last and has_diag:
                                mm = pe.matmul(PS[:, SCB, w - 128:w], ident[:, :], negtri[:, :], start=False, stop=True)
                            kb.end("pe", mm, reads=[rrB, diagB, cB], writes=[PSB[SCB]], partial=(h16 > 0))

                        for h16 in range(16):
                            pend.append(idx_x(h16))
                            if len(pend) > 1:
                                idx_acc(pend.pop(0))
                                yield
                        while pend:
                            idx_acc(pend.pop(0))
                        kb.op("dve", lambda: dve.tensor_copy(out=score[:, s0:s0 + w], in_=PS[:, SCB, 0:w]),
                              reads=[PSB[SCB]], writes=[scoreB], partial=(sc_i > 0))
                        yield
                    if dbg:
                        kb.dma("sp", DBG_SC[b, i, :, 0:L], score[:, 0:L], dbgB.dsem, reads=[scoreB], writes=[dbgB], partial=True)
                    bs, bsB = bs_r.next()
                    cB_, mxB, cntB, tB3, eB, txB = bsB[0], bsB[1], bsB[2], bsB[3], bsB[4], bsB[5]
                    thrB = mxB
                    if L <= TOPK:
                        kb.op("dve", lambda: dve.memset(bs[:, 1:2], -1e29), writes=[thrB])
                    else:
                        kb.op("dve", lambda: dve.tensor_reduce(out=bs[:, 1:2], in_=score[:, 0:L], axis=AX.X, op=ALU.max),
                              reads=[scoreB], writes=[mxB])
                        wk = BIS_W / 2.0
                        kb.op("dve", lambda: dve.tensor_scalar(out=bs[:, 0:1], in0=bs[:, 1:2], scalar1=-1.0, scalar2=BIS_W - wk,
                                                               op0=ALU.mult, op1=ALU.add), reads=[mxB], writes=[cB_])
                        thrS = 2.0 * TOPK - L - 0.5
                        for it in range(NBIS):
                            kb.op("act", lambda: act.activation(out=zbuf[:, 0:L], in_=score[:, 0:L], func=AF.Sign,
                                                                bias=bs[:, 0:1], scale=1.0, accum_out=bs[:, 2:3]),
                                  reads=[scoreB, cB_], writes=[zB, cntB])
                            kb.op("act", lambda: act.activation(out=bs[:, 3:4], in_=bs[:, 2:3], func=AF.Sign, bias=-thrS, scale=1.0),
                                  reads=[cntB], writes=[tB3])
                            kb.op("act", lambda: act.activation(out=bs[:, 0:1], in_=bs[:, 3:4], func=AF.Identity, scale=-wk / 2.0,
                                                                bias=bs[:, 0:1]),
                                  reads=[tB3, cB_], writes=[cB_])
                            wlast = wk
                            wk = wk / 2.0
                            yield
                        for _ in range(4):
                            yield
                        nB = bsB[6]
                        kb.op("act", lambda: act.activation(out=bs[:, 6:7], in_=bs[:, 0:1], func=AF.Identity, scale=1.0,
                                                            bias=wlast / 2.0), reads=[cB_], writes=[nB])
                        kb.op("dve", lambda: dve.tensor_scalar(out=bs[:, 1:2], in0=bs[:, 6:7], scalar1=-1.0, scalar2=None,
                                                               op0=ALU.mult), reads=[nB], writes=[thrB])
                        kb.op("act", lambda: act.activation(out=zbuf[:, 0:L], in_=score[:, 0:L], func=AF.Sign,
                                                            bias=bs[:, 6:7], scale=1.0, accum_out=bs[:, 2:3]),
                              reads=[scoreB, nB], writes=[zB, cntB])
                        kb.op("dve", lambda: dve.tensor_scalar(out=bs[:, 4:5], in0=bs[:, 2:3], scalar1=0.5, scalar2=L / 2.0 - TOPK,
                                                               op0=ALU.mult, op1=ALU.add), reads=[cntB], writes=[eB])
                        kb.op("dve", lambda: dve.tensor_scalar(out=bs[:, 4:5], in0=bs[:, 4:5], scalar1=7.0, scalar2=0.0,
                                                               op0=ALU.min, op1=ALU.max), reads=[eB], writes=[eB])
                        kb.op("dve", lambda: dve.tensor_scalar(out=zbuf[:, 0:L], in0=score[:, 0:L], scalar1=bs[:, 1:2], scalar2=-1e36,
                                                               op0=ALU.is_lt, op1=ALU.mult), reads=[scoreB, thrB], writes=[zB])
                        kb.op("dve", lambda: dve.scalar_tensor_tensor(out=zbuf[:, 0:L], in0=score[:, 0:L], scalar=-1.0,
                                                                     in1=zbuf[:, 0:L], op0=ALU.mult, op1=ALU.add),
                              reads=[scoreB, zB], writes=[zB])
                        z8, z8B = z8_r.next()
                        z8a, z8b, z8c = kb.buf(), kb.buf(), kb.buf()
                        kb.op("dve", lambda: dve.max(out=z8[:, 0:8], in_=zbuf[:, 0:L]), reads=[zB], writes=[z8B])
                        kb.op("dve", lambda: dve.tensor_scalar(out=z8[:, 8:16], in0=iota8[:, :], scalar1=bs[:, 4:5], scalar2=None,
                                                               op0=ALU.is_equal), reads=[smallB, eB, z8B], writes=[z8B])
                        kb.op("dve", lambda: dve.scalar_tensor_tensor(out=z8[:, 16:24], in0=z8[:, 8:16], scalar=-1.0, in1=z8[:, 0:8],
                                                                     op0=ALU.mult, op1=ALU.mult, accum_out=bs[:, 5:6]),
                              reads=[z8B], writes=[z8B, txB])
                        thrB = txB
                        if dbg:
                            kb.dma("sp", DBG_BS[b, i, :, 0:6], bs[:, 0:6], dbgB.dsem, reads=[txB, eB, cntB, mxB, cB_], writes=[dbgB], partial=True)
                            kb.dma("sp", DBG_Z8[b, i, :, :], z8[:, :], dbgB.dsem, reads=[z8B], writes=[dbgB], partial=True)
                        yield
                    tcol = 1 if L <= TOPK else 5
                    if dbg:
                        kb.dma("sp", DBG_THR[b, i, :, :], bs[:, tcol:tcol + 1], dbgB.dsem, reads=[thrB], writes=[dbgB], partial=True)
                    nm, nmB = nm_r.next()
                    kb.op("dve", lambda: dve.tensor_scalar(out=nm[:, 0:L], in0=score[:, 0:L], scalar1=bs[:, tcol:tcol + 1], scalar2=NEG,
                                                           op0=ALU.is_lt, op1=ALU.mult),
                          reads=[scoreB, thrB], writes=[nmB])
                    nm_of[i] = (nm, nmB)
                    yield

                def gen_battn(i):
                    nm, nmB = nm_of.pop(i)
                    qbt, qbtB = qbt_r.next()
                    gbt, gbtB = gbt_r.next()
                    kb.dma("sp", qbt[:], QBT[b].rearrange("c p t -> p c t")[:, :, i * 128:(i + 1) * 128], qbtB.dsem,
                           reads=[scrB[4]], writes=[qbtB])
                    kb.dma("sp", gbt[:], GB[b, i * 128:(i + 1) * 128, :], gbtB.dsem, reads=[scrB[5]], writes=[gbtB])
                    ng = (i + 4) // 4
                    units = [(h, g) for h in range(8) for g in range(ng)]
                    pend = []

                    def qk_exp_b(u):
                        h, g = u
                        js = list(range(4 * g, min(4 * g + 4, i + 1)))
                        bk = xcnt[0] % 4
                        xcnt[0] += 1
                        rd = [qbtB, ckvTB, nmB, xabB, cB]
                        kb.begin("pe", reads=rd, writes=[PSB[bk]])
                        for jj, j in enumerate(js):
                            near = j >= i - 1
                            o_ap = PS[:, bk, jj * 128:(jj + 1) * 128]
                            for cc in range(2):
                                mm = pe.matmul(o_ap, ckvT[:, cc, j * 128:(j + 1) * 128], qbt[:, 2 * h + cc, :],
                                               start=(cc == 0), stop=False)
                            mm = pe.matmul(o_ap, nm[:, j * 128:(j + 1) * 128], ident[:, :], start=False, stop=not near)
                            if near:
                                case = 0 if j == i else 1
                                mm = pe.matmul(o_ap, jmat[:, :], XAB[:, 8 + h, case, :], start=False, stop=True)
                        kb.end("pe", mm, reads=rd, writes=[PSB[bk]])
                        pt, ptB = pt_r.next()
                        w = len(js) * 128
                        kb.op("act", lambda: act.activation(out=pt[:, 0:w], in_=PS[:, bk, 0:w], func=AF.Exp),
                              reads=[PSB[bk]], writes=[ptB])
                        return (u, js, pt, ptB)

                    def pv_b(item):
                        (h, g), js, pt, ptB = item
                        ob = OB[h % 2]
                        kb.begin("pe", reads=[ptB, VBB], writes=[PSB[ob]])
                        for jj, j in enumerate(js):
                            mm = pe.matmul(PS[:, ob, 0:129], pt[:, jj * 128:(jj + 1) * 128], VB[:, j, h, :],
                                           start=(j == 0), stop=(j == i))
                        kb.end("pe", mm, reads=[ptB, VBB], writes=[PSB[ob]], partial=(g > 0))
                        if g == ng - 1:
                            rc, rcB = rc_r.next()
                            ot, otB = ot_r.next()
                            kb.op("dve", lambda: dve.reciprocal(out=rc[:], in_=PS[:, ob, 128:129]), reads=[PSB[ob]], writes=[rcB])
                            kb.op("dve", lambda: dve.scalar_tensor_tensor(out=ot[:], in0=PS[:, ob, 0:128], scalar=rc[:, 0:1],
                                                                         in1=gbt[:, h * 128:(h + 1) * 128], op0=ALU.mult,
                                                                         op1=ALU.mult),
                                  reads=[PSB[ob], rcB, gbtB], writes=[otB])
                            deferred.append((ot, otB, h, i))
                            run_deferred(DEFER3) if len(deferred) >= DEFER3 + 4 else None

                    for u in units:
                        pend.append(qk_exp_b(u))
                        if len(pend) > 1:
                            pv_b(pend.pop(0))
                            yield
                    while pend:
                        pv_b(pend.pop(0))
                        yield

                def n_steps_index(i):
                    L = (i + 1) * 128
                    nsc = (L + 511) // 512
                    return 1 + nsc * 16 + (0 if L <= TOPK else NBIS + 5) + 1

                def n_steps_battn(i):
                    return 8 * ((i + 4) // 4)

                LOOK = 2
                for i0 in range(min(LOOK, NT)):
                    for _ in gen_index(i0):
                        pass
                for i in range(NT):
                    ga = gen_battn(i)
                    na = n_steps_battn(i)
                    gi = gen_index(i + LOOK) if i + LOOK < NT else None
                    ni = n_steps_index(i + LOOK) if gi is not None else 0
                    da = di = 0
                    a_alive, i_alive = True, gi is not None
                    while a_alive or i_alive:
                        pick_a = a_alive and (not i_alive or da * ni <= di * na)
                        if pick_a:
                            try:
                                next(ga)
                                da += 1
                            except StopIteration:
                                a_alive = False
                        else:
                            try:
                                next(gi)
                                di += 1
                            except StopIteration:
                                i_alive = False
                run_deferred(0)
                if dbg:
                    kb.dma("sp", DBG_OT[b], oT[:], dbgB.dsem, reads=[oTB], writes=[dbgB], partial=True)
                kb.barrier()

            with ExitStack() as p4:
                kb.dma("sp", g_bc[:], bcast(gpost_d[0:1, :], D), gB.dsem, writes=[gB])
                wo = sb("wo", [128, 16, D], BF16, p4)
                woB = kb.buf("wo", dma=True)
                for q in range(4):
                    kb.dma("pool", wo[:, q * 4:(q + 1) * 4, :],
                           wout_d[q * 512:(q + 1) * 512, :].rearrange("(k p) n -> p k n", p=128),
                           woB.dsem, writes=[woB], partial=True)
                xt_r = Ring(kb, p4, nc, "xt4", [128, D], F32, 2, dma=True)
                yn_r = Ring(kb, p4, nc, "yn4", [128, D], F32, 2, dma=True)
                st_r = CRing(kb, p4, nc, "st4", 4, 2)
                for i in range(NT):
                    xt, xtB = xt_r.next()
                    kb.dma("sp", xt[:], x_d[b, i * 128:(i + 1) * 128, :], xtB.dsem, writes=[xtB])
                    g4 = (i % 2) * 4
                    for n4 in range(4):
                        bk = g4 + n4
                        kb.begin("pe", reads=[oTB, woB], writes=[PSB[bk]])
                        for k in range(16):
                            mm = pe.matmul(PS[:, bk, :], oT[:, k, i * 128:(i + 1) * 128], wo[:, k, n4 * 512:(n4 + 1) * 512],
                                           start=(k == 0), stop=(k == 15))
                        kb.end("pe", mm, reads=[oTB, woB], writes=[PSB[bk]])
                    ybanks = [PSB[g4 + n] for n in range(4)]
                    yap = PS[:, g4:g4 + 4, :]
                    st, sB_ = st_r.next()
                    ssB, vB, rB = sB_[0], sB_[1], sB_[2]
                    yn, ynB = yn_r.next()
                    kb.op("act", lambda: act.activation(out=yn[:].rearrange("p (a n) -> p a n", a=4), in_=yap, func=AF.Square,
                                                        accum_out=st[:, 0:1]),
                          reads=ybanks, writes=[ynB, ssB])
                    rstd_ops(st[:, 0:1], ssB, st[:, 2:3], rB, 1.0 / D, st[:, 1:2], vB)
                    kb.op("dve", lambda: dve.scalar_tensor_tensor(out=yn[:].rearrange("p (a n) -> p a n", a=4), in0=yap,
                                                                 scalar=st[:, 2:3],
                                                                 in1=g_bc[:].rearrange("p (a n) -> p a n", a=4),
                                                                 op0=ALU.mult, op1=ALU.mult),
                          reads=ybanks + [rB, gB], writes=[ynB])
                    kb.op("dve", lambda: dve.tensor_tensor(out=yn[:], in0=yn[:], in1=xt[:], op=ALU.add),
                          reads=[ynB, xtB], writes=[ynB])
                    kb.dma("sp", out_d[b, i * 128:(i + 1) * 128, :], yn[:], ynB.dsem, reads=[ynB], writes=[])
                kb.barrier()
            p234.close()
        print("build: waits=%d counts=%s" % (kb.nwaits, {k: v for k, v in kb.cnt.items() if k in kb.E}))
    return nc


_NC_CACHE = {}


def _get_nc(T, NSEQ, dbg=False):
    key = (T, NSEQ, dbg)
    if key not in _NC_CACHE:
        _NC_CACHE[key] = build_nc(T, NSEQ, dbg)
    return _NC_CACHE[key]


def make_in_maps(inputs, n_cores, nseq):
    c = _consts()
    f = lambda a: np.ascontiguousarray(np.asarray(a, dtype=np.float32))
    shared = {
        "w_in": f(inputs["w_in"])[0],
        "w_out": f(inputs["w_out"])[0],
        "norm_pre_g": f(inputs["norm_pre_g"]),
        "norm_post_g": f(inputs["norm_post_g"]),
        "lambda_q1": f(inputs["lambda_q1"]),
        "lambda_k1": f(inputs["lambda_k1"]),
        "lambda_q2": f(inputs["lambda_q2"]),
        "lambda_k2": f(inputs["lambda_k2"]),
        "subln_g": f(inputs["subln_g"]),
        "kv_norm_g": f(inputs["kv_norm_g"]),
        "idx_k_norm_g": f(inputs["idx_k_norm_g"]),
        "w_uv": f(inputs["w_uv"])[0],
        "rel_bias": f(inputs["rel_bias"]),
    }
    shared.update(c)
    x = f(inputs["x"])
    maps = []
    for i in range(n_cores):
        m = dict(shared)
        m["x"] = np.ascontiguousarray(x[i * nseq:(i + 1) * nseq])
        maps.append(m)
    return maps


def kernel(**inputs):
    x = np.asarray(inputs["x"])
    B, T, _ = x.shape
    n_cores = 8
    nseq = B // n_cores
    nc = _get_nc(T, nseq)
    in_maps = make_in_maps(inputs, n_cores, nseq)
    res = run_bass_kernel_spmd(nc, in_maps, core_ids=list(range(n_cores)))
    out = np.concatenate([np.asarray(r["out"]) for r in res.results], axis=0)
    return out.astype(np.float32)
```

```python
import math
from contextlib import ExitStack

import numpy as np
import ml_dtypes

import concourse.bass as bass
import concourse.mybir as mybir
from concourse.bass_utils import run_bass_kernel_spmd

F32 = mybir.dt.float32
BF16 = mybir.dt.bfloat16
AF = mybir.ActivationFunctionType
ALU = mybir.AluOpType
AX = mybir.AxisListType

D = 2048
N_IN = 8528
EPS = 1e-6
NEG = -30000.0
NBIS = 14
BIS_W = 256.0
import os as _os
DEFER3 = int(_os.environ.get('K_DEFER3', '4'))
DEFER2 = int(_os.environ.get('K_DEFER2', '1'))


class Buf:
    __slots__ = ("w", "r", "name", "dsem", "excl")

    def __init__(self, name="", dsem=None, excl=False):
        self.w = {}
        self.r = {}
        self.name = name
        self.dsem = dsem
        self.excl = excl


class KB:
    def __init__(self, nc, es):
        self.nc = nc
        self.es = es
        self.E = {"pe": nc.tensor, "act": nc.scalar, "dve": nc.vector, "pool": nc.gpsimd, "sp": nc.sync}
        self.sem = {}
        self.cnt = {}
        self.seen = {e: {} for e in self.E}
        for e in self.E:
            self.sem[e] = es.enter_context(nc.semaphore("s_" + e))
            self.cnt[e] = 0
        self.nd = 0
        self.nwaits = 0

    def new_dsem(self):
        key = "d%d" % self.nd
        self.nd += 1
        self.sem[key] = self.es.enter_context(self.nc.semaphore(key))
        self.cnt[key] = 0
        return key

    def buf(self, name="", dma=False):
        return Buf(name, self.new_dsem() if dma else None)

    def _wait(self, e, key, val):
        if self.seen[e].get(key, 0) >= val:
            return
        self.E[e].wait_ge(self.sem[key], val)
        self.seen[e][key] = val
        self.nwaits += 1

    def begin(self, e, reads=(), writes=(), partial=False):
        deps = {}

        def add(k, v):
            if k == e and e == "pe":
                return
            if deps.get(k, 0) < v:
                deps[k] = v

        for b in reads:
            for k, v in b.w.items():
                add(k, v)
            if b.excl:
                for k, v in b.r.items():
                    if k != e:
                        add(k, v)
        for b in writes:
            for k, v in b.w.items():
                if not (k == e and partial):
                    add(k, v)
            for k, v in b.r.items():
                add(k, v)
        for k, v in deps.items():
            self._wait(e, k, v)

    def _mark(self, k, v, reads, writes, partial):
        for b in reads:
            if b.r.get(k, 0) < v:
                b.r[k] = v
        for b in writes:
            if partial:
                if b.w.get(k, 0) < v:
                    b.w[k] = v
            else:
                b.w = {k: v}
                b.r = {}

    def end(self, e, inst, reads=(), writes=(), partial=False):
        self.cnt[e] += 1
        inst.then_inc(self.sem[e], 1)
        self._mark(e, self.cnt[e], reads, writes, partial)

    def op(self, e, fn, reads=(), writes=(), partial=False):
        self.begin(e, reads, writes, partial)
        inst = fn()
        self.end(e, inst, reads, writes, partial)

    def dma(self, q, out, in_, dsem, reads=(), writes=(), partial=False):
        self.begin(q, reads, writes, partial)
        inst = self.E[q].dma_start(out=out, in_=in_)
        self.cnt[dsem] += 16
        inst.then_inc(self.sem[dsem], 16)
        self._mark(dsem, self.cnt[dsem], reads, writes, partial)

    def barrier(self):
        for e in self.E:
            for k in self.sem:
                if k != e and self.cnt[k] > 0:
                    self._wait(e, k, self.cnt[k])


class Ring:
    uid = 0

    def __init__(self, kb, es, nc, name, shape, dtype, n, dma=False):
        Ring.uid += 1
        self.t = [es.enter_context(nc.sbuf_tensor("%s%d_r%d" % (name, i, Ring.uid), shape, dtype)) for i in range(n)]
        self.b = [kb.buf("%s%d" % (name, i), dma) for i in range(n)]
        self.i = -1
        self.n = n

    def next(self):
        self.i = (self.i + 1) % self.n
        return self.t[self.i], self.b[self.i]


class CRing:
    def __init__(self, kb, es, nc, name, ncols, n):
        Ring.uid += 1
        self.t = [es.enter_context(nc.sbuf_tensor("%s%d_c%d" % (name, i, Ring.uid), [128, ncols], F32)) for i in range(n)]
        self.b = [[kb.buf("%s%d_%d" % (name, i, c)) for c in range(ncols)] for i in range(n)]
        self.i = -1
        self.n = n

    def next(self):
        self.i = (self.i + 1) % self.n
        return self.t[self.i], self.b[self.i]


def _t5_bucket_np(n):
    n = np.maximum(n, 0)
    nf = np.maximum(n, 1).astype(np.float32)
    large = 16 + (np.log(nf / np.float32(16)) / np.float32(math.log(128 / 16)) * np.float32(16)).astype(np.int32)
    large = np.minimum(large, 31)
    return np.where(n < 16, n, large)


def _consts():
    ident = np.eye(128, dtype=np.float32)
    jmat = ident[::-1].copy()
    oh = np.zeros((33, 510), np.float32)
    for case, delta in enumerate((0, 128)):
        for u in range(255):
            n = delta - 127 + u
            if n < 0:
                oh[32, case * 255 + u] = 1.0
            else:
                oh[int(_t5_bucket_np(np.array([n]))[0]), case * 255 + u] = 1.0
    negtri = np.where(np.arange(128)[None, :] > np.arange(128)[:, None], -1e30, 0.0).astype(np.float32)
    return {
        "ident_bf": ident.astype(ml_dtypes.bfloat16),
        "jmat_bf": jmat.astype(ml_dtypes.bfloat16),
        "oh": oh,
        "negtri_bf": negtri.astype(ml_dtypes.bfloat16),
    }


def build_nc(T=2048, NSEQ=2, dbg=False):
    NT = T // 128
    NTC = T // 512
    TOPK = min(256, T // 4)
    nc = bass.Bass("TRN2", target_bir_lowering=False)

    def din(name, shape, dt=F32):
        return nc.dram_tensor(name, list(shape), dt, kind="ExternalInput").ap()

    x_d = din("x", [NSEQ, T, D])
    win_d = din("w_in", [D, N_IN])
    wout_d = din("w_out", [D, D])
    gpre_d = din("norm_pre_g", [1, D])
    gpost_d = din("norm_post_g", [1, D])
    lq1_d = din("lambda_q1", [1, 64])
    lk1_d = din("lambda_k1", [1, 64])
    lq2_d = din("lambda_q2", [1, 64])
    lk2_d = din("lambda_k2", [1, 64])
    gsub_d = din("subln_g", [1, 128])
    gkv_d = din("kv_norm_g", [1, 256])
    gik_d = din("idx_k_norm_g", [1, 64])
    wuv_d = din("w_uv", [8, 256, 128])
    relb_d = din("rel_bias", [32, 16])
    ident_d = din("ident_bf", [128, 128], BF16)
    jmat_d = din("jmat_bf", [128, 128], BF16)
    oh_d = din("oh", [33, 510])
    negtri_d = din("negtri_bf", [128, 128], BF16)
    out_d = nc.dram_tensor("out", [NSEQ, T, D], F32, kind="ExternalOutput").ap()

    skind = "ExternalOutput" if dbg else "Internal"

    def dscr(name, shape, dt=BF16):
        return nc.dram_tensor(name, list(shape), dt, kind=skind).ap()

    QAT = dscr("s_qat", [NSEQ, 8, 128, T])
    KAT = dscr("s_kat", [NSEQ, 8, 128, T])
    VA = dscr("s_va", [NSEQ, T, 1024])
    GA = dscr("s_ga", [NSEQ, T, 1024])
    QBT = dscr("s_qbt", [NSEQ, 16, 128, T])
    GB = dscr("s_gb", [NSEQ, T, 1024])
    IQT = dscr("s_iqt", [NSEQ, 8, 128, T])
    GD = dscr("s_gd", [16, 510])
    if dbg:
        DBG_CKVT = dscr("s_ckvt", [NSEQ, 128, 2, T])
        DBG_IKT = dscr("s_ikt", [NSEQ, 128, 2, T])
        DBG_OT = dscr("s_ot", [NSEQ, 128, 16, T])
        DBG_THR = dscr("s_thr", [NSEQ, NT, 128, 1], F32)
        DBG_SC = dscr("s_sc", [NSEQ, NT, 128, T], F32)
        DBG_BS = dscr("s_bs", [NSEQ, NT, 128, 8], F32)
        DBG_Z8 = dscr("s_z8", [NSEQ, NT, 128, 24], F32)

    es = ExitStack()
    with es:
        kb = KB(nc, es)
        E = kb.E
        pe, act, dve, pool, sp = E["pe"], E["act"], E["dve"], E["pool"], E["sp"]

        uid = [0]

        def sb(name, shape, dt, stack=es):
            uid[0] += 1
            return stack.enter_context(nc.sbuf_tensor("%s_u%d" % (name, uid[0]), list(shape), dt))

        PS = es.enter_context(nc.psum_tensor("ps", [128, 8, 512], F32))
        PSB = [Buf("ps%d" % i, excl=True) for i in range(8)]
        bank_rr = [0]

        def nextbank(lo=0, hi=8):
            b = lo + bank_rr[0] % (hi - lo)
            bank_rr[0] += 1
            return b

        ident = sb("ident", [128, 128], BF16)
        jmat = sb("jmat", [128, 128], BF16)
        negtri = sb("negtri", [128, 128], BF16)
        oh_sb = sb("oh_sb", [33, 510], F32)
        cB = kb.buf("consts", dma=True)
        kb.dma("sp", ident[:], ident_d[:, :], cB.dsem, writes=[cB], partial=True)
        kb.dma("sp", jmat[:], jmat_d[:, :], cB.dsem, writes=[cB], partial=True)
        kb.dma("sp", negtri[:], negtri_d[:, :], cB.dsem, writes=[cB], partial=True)
        kb.dma("sp", oh_sb[:], oh_d[:, :], cB.dsem, writes=[cB], partial=True)

        def bcast(src2d, n):
            return bass.AP(src2d.tensor, src2d.offset, [[0, 128], [1, n]])

        g_bc = sb("g_bc", [128, D], F32)
        gB = kb.buf("g_bc", dma=True)
        gsub_bc = sb("gsub_bc", [128, 128], F32)
        gkv_bc = sb("gkv_bc", [128, 256], F32)
        gik_bc = sb("gik_bc", [128, 64], F32)
        lamt = sb("lamt", [128, 4, 64], F32)
        kb.dma("sp", gsub_bc[:], bcast(gsub_d[0:1, :], 128), cB.dsem, writes=[cB], partial=True)
        kb.dma("sp", gkv_bc[:], bcast(gkv_d[0:1, :], 256), cB.dsem, writes=[cB], partial=True)
        kb.dma("sp", gik_bc[:], bcast(gik_d[0:1, :], 64), cB.dsem, writes=[cB], partial=True)
        for i, l_d in enumerate((lq1_d, lk1_d, lq2_d, lk2_d)):
            kb.dma("sp", lamt[:, i, :], bcast(l_d[0:1, :], 64), cB.dsem, writes=[cB], partial=True)
        wuv = sb("wuv", [128, 8, 2, 128], BF16)
        wuvB = kb.buf("wuv", dma=True)
        kb.dma("pool", wuv[:], wuv_d.rearrange("h (cc p) n -> p h cc n", p=128), wuvB.dsem, writes=[wuvB])

        mhalf = sb("mhalf", [128, 1], F32)
        neg_lam = sb("neg_lam", [128, 1], F32)
        smallB = kb.buf("small")
        kb.op("dve", lambda: dve.memset(mhalf[:], -0.5), writes=[smallB], partial=True)
        iota8 = sb("iota8", [128, 8], F32)
        for c8 in range(8):
            kb.op("dve", lambda: dve.memset(iota8[:, c8:c8 + 1], float(c8)), writes=[smallB], partial=True)
        lprod = sb("lprod", [128, 2, 64], F32)
        lsum = sb("lsum", [128, 2], F32)
        lexp = sb("lexp", [128, 2], F32)
        lB = kb.buf("lamb")
        kb.op("dve", lambda: dve.tensor_tensor(out=lprod[:, 0, :], in0=lamt[:, 0, :], in1=lamt[:, 1, :], op=ALU.mult),
              reads=[cB], writes=[lB], partial=True)
        kb.op("dve", lambda: dve.tensor_tensor(out=lprod[:, 1, :], in0=lamt[:, 2, :], in1=lamt[:, 3, :], op=ALU.mult),
              reads=[cB], writes=[lB], partial=True)
        lB2 = kb.buf("lamb2")
        kb.op("dve", lambda: dve.tensor_reduce(out=lsum[:], in_=lprod[:], axis=AX.X, op=ALU.add), reads=[lB], writes=[lB2])
        lB3 = kb.buf("lamb3")
        kb.op("act", lambda: act.activation(out=lexp[:], in_=lsum[:], func=AF.Exp), reads=[lB2], writes=[lB3])
        lB4 = kb.buf("lamb4")
        ltmp = sb("ltmp", [128, 1], F32)
        kb.op("dve", lambda: dve.tensor_tensor(out=ltmp[:], in0=lexp[:, 1:2], in1=lexp[:, 0:1], op=ALU.subtract),
              reads=[lB3], writes=[lB4])
        kb.op("dve", lambda: dve.tensor_scalar(out=neg_lam[:], in0=ltmp[:], scalar1=-0.2, scalar2=None, op0=ALU.add),
              reads=[lB4], writes=[smallB], partial=True)
        gsB = kb.buf("gsub")
        kb.op("dve", lambda: dve.tensor_scalar(out=gsub_bc[:], in0=gsub_bc[:], scalar1=0.8, scalar2=None, op0=ALU.mult),
              reads=[cB], writes=[gsB])

        tab33 = sb("tab33", [33, 16], F32)
        r31 = sb("r31", [32, 16], F32)
        tB = kb.buf("tab", dma=True)
        kb.dma("sp", tab33[0:32, :], relb_d[:, :], tB.dsem, writes=[tB], partial=True)
        kb.dma("sp", r31[:], bass.AP(relb_d.tensor, relb_d[31:32, :].offset, [[0, 32], [1, 16]]), tB.dsem,
               writes=[tB], partial=True)
        tB2 = kb.buf("tab2")
        kb.op("dve", lambda: dve.tensor_tensor(out=tab33[0:32, :], in0=tab33[0:32, :], in1=r31[:], op=ALU.subtract),
              reads=[tB], writes=[tB2], partial=True)
        kb.op("dve", lambda: dve.memset(tab33[32:33, :], NEG), writes=[tB2], partial=True)
        gd_sb = sb("gd_sb", [16, 510], BF16)
        gdB = kb.buf("gd", dma=True)
        kb.begin("pe", reads=[tB2, cB], writes=[PSB[0]])
        mm = pe.matmul(PS[0:16, 0, 0:510], tab33[:, :], oh_sb[:, :], start=True, stop=True)
        kb.end("pe", mm, reads=[tB2, cB], writes=[PSB[0]])
        kb.op("act", lambda: act.activation(out=gd_sb[:], in_=PS[0:16, 0, 0:510], func=AF.Copy), reads=[PSB[0]], writes=[gdB])
        GDB = kb.buf("GD", dma=True)
        kb.dma("sp", GD[:, :], gd_sb[:], gdB.dsem, reads=[gdB], writes=[GDB])
        XAB = sb("xab", [128, 16, 2, 128], BF16)
        xabB = kb.buf("xab", dma=True)
        for case in range(2):
            kb.dma("sp", XAB[:, :, case, :],
                   bass.AP(GD.tensor, GD.offset + case * 255, [[1, 128], [510, 16], [1, 128]]),
                   xabB.dsem, reads=[GDB], writes=[xabB], partial=True)

        ckvT = sb("ckvT", [128, 2, T], BF16)
        ikT2 = sb("ikT2", [128, 2, T], BF16)
        iw_sb = sb("iw_sb", [128, NT, 16], F32)
        ckvTB = kb.buf("ckvT")
        ikTB = kb.buf("ikT")
        iwB = kb.buf("iw")
        oTB = kb.buf("oT")

        def rstd_ops(ss_ap, ssB, out_t, outB, inv_n, tmp_t, tmpB):
            kb.op("act", lambda: act.activation(out=tmp_t, in_=ss_ap, func=AF.Ln, scale=inv_n, bias=EPS),
                  reads=[ssB], writes=[tmpB])
            kb.op("act", lambda: act.activation(out=out_t, in_=tmp_t, func=AF.Exp, scale=-0.5),
                  reads=[tmpB], writes=[outB])

        for b in range(NSEQ):
            with ExitStack() as p1:
                hT = sb("hT", [128, 16, T], BF16, p1)
                hTB = [kb.buf("hT%d" % i) for i in range(NTC)]
                xt_r = Ring(kb, p1, nc, "xt", [128, D], F32, 2, dma=True)
                hb_r = Ring(kb, p1, nc, "hb", [128, D], BF16, 2)
                junk = sb("junk1", [128, D], BF16, p1)
                junkB = kb.buf("junk")
                st_r = CRing(kb, p1, nc, "st1", 6, 4)
                wt_r = Ring(kb, p1, nc, "wt", [128, 16, 512], BF16, 3, dma=True)
                fst_r = Ring(kb, p1, nc, "fst", [128, T], BF16, 2, dma=True)
                tst_r = Ring(kb, p1, nc, "tst", [128, 512], BF16, 3, dma=True)
                ef_r = Ring(kb, p1, nc, "ef", [128, 512], F32, 2)
                ckn_r = Ring(kb, p1, nc, "ckn", [128, 512], BF16, 2)
                for t_, tB_ in zip(ckn_r.t, ckn_r.b):
                    kb.op("dve", lambda: dve.memset(t_[:, 320:448], 0.0), writes=[tB_], partial=True)
                scrB = [kb.buf("scr%d" % i, dma=True) for i in range(8)]

                kb.dma("sp", g_bc[:], bcast(gpre_d[0:1, :], D), gB.dsem, writes=[gB])
                def stage_a(tb):
                    xt, xtB = xt_r.next()
                    kb.dma("sp", xt[:], x_d[b, tb * 128:(tb + 1) * 128, :], xtB.dsem, writes=[xtB])
                    st, sB_ = st_r.next()
                    ssB, vB, rB = sB_[0], sB_[1], sB_[2]
                    kb.op("act", lambda: act.activation(out=junk[:], in_=xt[:], func=AF.Square, accum_out=st[:, 0:1]),
                          reads=[xtB], writes=[junkB, ssB])
                    rstd_ops(st[:, 0:1], ssB, st[:, 2:3], rB, 1.0 / D, st[:, 1:2], vB)
                    hb, hbB = hb_r.next()
                    kb.op("dve", lambda: dve.scalar_tensor_tensor(out=hb[:], in0=xt[:], scalar=st[:, 2:3], in1=g_bc[:],
                                                                 op0=ALU.mult, op1=ALU.mult),
                          reads=[xtB, rB, gB], writes=[hbB])
                    return hb, hbB

                def stage_b(tb, hb, hbB):
                    for q in range(4):
                        bk = nextbank()
                        kb.begin("pe", reads=[hbB, cB], writes=[PSB[bk]])
                        for k4 in range(4):
                            k = q * 4 + k4
                            mm = pe.matmul(PS[:, bk, k4 * 128:(k4 + 1) * 128], hb[:, k * 128:(k + 1) * 128], ident[:, :],
                                           start=True, stop=True)
                        kb.end("pe", mm, reads=[hbB, cB], writes=[PSB[bk]])
                        src = PS[:, bk, :].rearrange("p (k n) -> p k n", k=4)
                        dst = hT[:, q * 4:(q + 1) * 4, tb * 128:(tb + 1) * 128]
                        if q % 2 == 0:
                            kb.op("act", lambda: act.activation(out=dst, in_=src, func=AF.Copy),
                                  reads=[PSB[bk]], writes=[hTB[tb // 4]], partial=True)
                        else:
                            kb.op("dve", lambda: dve.tensor_copy(out=dst, in_=src),
                                  reads=[PSB[bk]], writes=[hTB[tb // 4]], partial=True)

                prev_a = None
                for tb in range(NT):
                    cur = stage_a(tb)
                    if prev_a is not None:
                        stage_b(tb - 1, *prev_a)
                    prev_a = cur
                stage_b(NT - 1, *prev_a)

                def load_w(cols):
                    wt, wtB = wt_r.next()
                    off = 0
                    first = True
                    for (c0, w) in cols:
                        kb.dma("pool", wt[:, :, off:off + w],
                               win_d[:, c0:c0 + w].rearrange("(k p) n -> p k n", p=128),
                               wtB.dsem, writes=[wtB], partial=not first)
                        first = False
                        off += w
                    return wt, wtB

                def feat_group(wt, wtB, c0, dst, chunk0, scale, sB):
                    for m in range(4):
                        stg, stgB = fst_r.next()
                        for tc in range(NTC):
                            bk = nextbank()
                            kb.begin("pe", reads=[wtB, hTB[tc]], writes=[PSB[bk]])
                            for k in range(16):
                                mm = pe.matmul(PS[:, bk, :], wt[:, k, m * 128:(m + 1) * 128], hT[:, k, tc * 512:(tc + 1) * 512],
                                               start=(k == 0), stop=(k == 15))
                            kb.end("pe", mm, reads=[wtB, hTB[tc]], writes=[PSB[bk]])
                            o_ap = stg[:, tc * 512:(tc + 1) * 512]
                            i_ap = PS[:, bk, :]
                            kb.op("act", lambda: act.activation(out=o_ap, in_=i_ap, func=AF.Copy, scale=scale),
                                  reads=[PSB[bk]], writes=[stgB], partial=(tc > 0))
                        kb.dma("sp", dst[b, chunk0 + m, :, :], stg[:], stgB.dsem, reads=[stgB], writes=[sB], partial=True)

                def tok_group(wt, wtB, c0, dst, dcol0, gate, sB):
                    for tb in range(NT):
                        bk = nextbank()
                        kb.begin("pe", reads=[wtB, hTB[tb // 4]], writes=[PSB[bk]])
                        for k in range(16):
                            mm = pe.matmul(PS[:, bk, :], hT[:, k, tb * 128:(tb + 1) * 128], wt[:, k, :],
                                           start=(k == 0), stop=(k == 15))
                        kb.end("pe", mm, reads=[wtB, hTB[tb // 4]], writes=[PSB[bk]])
                        stg, stgB = tst_r.next()
                        if not gate:
                            kb.op("dve", lambda: dve.tensor_copy(out=stg[:], in_=PS[:, bk, :]), reads=[PSB[bk]], writes=[stgB])
                        else:
                            ef, efB = ef_r.next()
                            kb.op("act", lambda: act.activation(out=ef[:], in_=PS[:, bk, :], func=AF.Exp, scale=-1.0),
                                  reads=[PSB[bk]], writes=[efB])
                            kb.op("act", lambda: act.activation(out=ef[:], in_=ef[:], func=AF.Ln, bias=1.0), reads=[efB], writes=[efB])
                            kb.op("act", lambda: act.activation(out=ef[:], in_=ef[:], func=AF.Exp, scale=-1.0),
                                  reads=[efB], writes=[efB])
                            kb.op("dve", lambda: dve.tensor_tensor(out=stg[:], in0=PS[:, bk, :], in1=ef[:], op=ALU.mult),
                                  reads=[PSB[bk], efB], writes=[stgB])
                        kb.dma("sp", dst[b, tb * 128:(tb + 1) * 128, dcol0:dcol0 + 512], stg[:], stgB.dsem,
                               reads=[stgB], writes=[sB], partial=True)

                def lat_group(wt, wtB):
                    for tb in range(NT):
                        bk = nextbank()
                        kb.begin("pe", reads=[wtB, hTB[tb // 4]], writes=[PSB[bk]])
                        for k in range(16):
                            mm = pe.matmul(PS[:, bk, 0:336], hT[:, k, tb * 128:(tb + 1) * 128], wt[:, k, 0:336],
                                           start=(k == 0), stop=(k == 15))
                        kb.end("pe", mm, reads=[wtB, hTB[tb // 4]], writes=[PSB[bk]])
                        st, sB_ = st_r.next()
                        kb.op("act", lambda: act.activation(out=junk[:, 0:256], in_=PS[:, bk, 0:256], func=AF.Square,
                                                            accum_out=st[:, 0:1]),
                              reads=[PSB[bk]], writes=[junkB, sB_[0]])
                        kb.op("act", lambda: act.activation(out=junk[:, 256:320], in_=PS[:, bk, 256:320], func=AF.Square,
                                                            accum_out=st[:, 1:2]),
                              reads=[PSB[bk]], writes=[junkB, sB_[1]])
                        rstd_ops(st[:, 0:1], sB_[0], st[:, 4:5], sB_[4], 1.0 / 256, st[:, 2:3], sB_[2])
                        rstd_ops(st[:, 1:2], sB_[1], st[:, 5:6], sB_[5], 1.0 / 64, st[:, 3:4], sB_[3])
                        ckn, cknB = ckn_r.next()
                        kb.op("dve", lambda: dve.scalar_tensor_tensor(out=ckn[:, 0:256], in0=PS[:, bk, 0:256], scalar=st[:, 4:5],
                                                                     in1=gkv_bc[:], op0=ALU.mult, op1=ALU.mult),
                              reads=[PSB[bk], sB_[4], cB], writes=[cknB], partial=True)
                        for hh in range(2):
                            kb.op("dve", lambda: dve.scalar_tensor_tensor(out=ckn[:, 256 + hh * 192:320 + hh * 192],
                                                                         in0=PS[:, bk, 256:320], scalar=st[:, 5:6],
                                                                         in1=gik_bc[:], op0=ALU.mult, op1=ALU.mult),
                                  reads=[PSB[bk], sB_[5], cB], writes=[cknB], partial=True)
                        kb.op("dve", lambda: dve.tensor_copy(out=iw_sb[:, tb, :], in_=PS[:, bk, 320:336]),
                              reads=[PSB[bk]], writes=[iwB], partial=True)
                        bk2 = nextbank()
                        kb.begin("pe", reads=[cknB, cB], writes=[PSB[bk2]])
                        for j in range(4):
                            mm = pe.matmul(PS[:, bk2, j * 128:(j + 1) * 128], ckn[:, j * 128:(j + 1) * 128], ident[:, :],
                                           start=True, stop=True)
                        kb.end("pe", mm, reads=[cknB, cB], writes=[PSB[bk2]])
                        kb.op("act", lambda: act.activation(out=ckvT[:, :, tb * 128:(tb + 1) * 128],
                                                            in_=PS[:, bk2, 0:256].rearrange("p (c n) -> p c n", c=2), func=AF.Copy),
                              reads=[PSB[bk2]], writes=[ckvTB], partial=True)
                        kb.op("act", lambda: act.activation(out=ikT2[:, :, tb * 128:(tb + 1) * 128],
                                                            in_=PS[:, bk2, 256:512].rearrange("p (c n) -> p c n", c=2),
                                                            func=AF.Copy),
                              reads=[PSB[bk2]], writes=[ikTB], partial=True)

                specs = [
                    ("f", [(0, 512)], (0, QAT, 0, 0.125, scrB[0])),
                    ("f", [(512, 512)], (512, QAT, 4, 0.125, scrB[0])),
                    ("f", [(1024, 512)], (1024, KAT, 0, 1.0, scrB[1])),
                    ("f", [(1536, 512)], (1536, KAT, 4, 1.0, scrB[1])),
                    ("t", [(2048, 512)], (2048, VA, 0, False, scrB[2])),
                    ("t", [(2560, 512)], (2560, VA, 512, False, scrB[2])),
                    ("t", [(3072, 512)], (3072, GA, 0, True, scrB[3])),
                    ("t", [(3584, 512)], (3584, GA, 512, True, scrB[3])),
                ]
                for g in range(4):
                    specs.append(("f", [(4096 + g * 512, 512)], (4096 + g * 512, QBT, g * 4, 0.0625, scrB[4])))
                specs.append(("l", [(6144, 256), (8448, 80)], ()))
                specs += [
                    ("t", [(6400, 512)], (6400, GB, 0, True, scrB[5])),
                    ("t", [(6912, 512)], (6912, GB, 512, True, scrB[5])),
                    ("f", [(7424, 512)], (7424, IQT, 0, 1.0, scrB[6])),
                    ("f", [(7936, 512)], (7936, IQT, 4, 1.0, scrB[6])),
                ]
                loaded = {}
                for gi, (kind, cols, args) in enumerate(specs):
                    for a in range(gi, min(gi + 3, len(specs))):
                        if a not in loaded:
                            loaded[a] = load_w(specs[a][1])
                    wt, wtB = loaded.pop(gi)
                    if kind == "f":
                        feat_group(wt, wtB, *args)
                    elif kind == "t":
                        tok_group(wt, wtB, *args)
                    else:
                        lat_group(wt, wtB)
                if dbg:
                    dbgB = kb.buf("dbg", dma=True)
                    kb.dma("sp", DBG_CKVT[b], ckvT[:], dbgB.dsem, reads=[ckvTB], writes=[dbgB], partial=True)
                    kb.dma("sp", DBG_IKT[b], ikT2[:], dbgB.dsem, reads=[ikTB], writes=[dbgB], partial=True)
                kb.barrier()

            p234 = ExitStack()
            p234.__enter__()
            oT = sb("oT", [128, 16, T], BF16, p234)
            with ExitStack() as p2:
                qt_r = Ring(kb, p2, nc, "qt", [128, 2, T], BF16, 2, dma=True)
                kt_r = Ring(kb, p2, nc, "kt", [128, 2, 2, T], BF16, 2, dma=True)
                for t_, tB_ in zip(kt_r.t, kt_r.b):
                    kb.op("dve", lambda: dve.memset(t_[:], 0.0), writes=[tB_], partial=True)
                vt_r = Ring(kb, p2, nc, "vt", [128, NT, 2, 129], BF16, 2, dma=True)
                gt_r = Ring(kb, p2, nc, "gt", [128, NT, 256], BF16, 2, dma=True)
                pt_r = Ring(kb, p2, nc, "pt", [128, 512], BF16, 4)
                o1_r = Ring(kb, p2, nc, "o1", [128, 128], F32, 4)
                o2_r = Ring(kb, p2, nc, "o2", [128, 128], F32, 4)
                ot_r = Ring(kb, p2, nc, "ot", [128, 128], BF16, 4)
                sc_r = CRing(kb, p2, nc, "sc2", 8, 4)
                junk2 = sb("junk2", [128, 128], F32, p2)
                junk2B = kb.buf("junk2")
                for t_, tB_ in zip(vt_r.t, vt_r.b):
                    kb.op("dve", lambda: dve.memset(t_[:, :, :, 128:129], 1.0), writes=[tB_], partial=True)
                TB_ = 7
                trc = [0]
                defer2 = []
                tile_no = [0]
                for hp in range(4):
                    qt, qtB = qt_r.next()
                    kt, ktB = kt_r.next()
                    vt, vtB = vt_r.next()
                    gt, gtB = gt_r.next()
                    qv = QAT[b].rearrange("(c m) p t -> m p c t", c=2)
                    kv = KAT[b].rearrange("(c m) p t -> m p c t", c=2)
                    kb.dma("sp", qt[:], qv[hp], qtB.dsem, reads=[scrB[0]], writes=[qtB])
                    for hh_ in range(2):
                        kb.dma("sp", kt[hh_ * 64:(hh_ + 1) * 64, :, hh_, :], kv[hp][hh_ * 64:(hh_ + 1) * 64], ktB.dsem,
                               reads=[scrB[1]], writes=[ktB], partial=True)
                    vv = VA[b].rearrange("(j p) (h d) -> p j h d", p=128, d=128)
                    for hh_ in range(2):
                        kb.dma("sp", vt[:, :, hh_, 0:128], vv[:, :, 2 * hp + hh_, :], vtB.dsem, reads=[scrB[2]], writes=[vtB],
                               partial=True)
                    gv = GA[b].rearrange("(j p) c -> p j c", p=128)
                    kb.dma("sp", gt[:], gv[:, :, hp * 256:(hp + 1) * 256], gtB.dsem, reads=[scrB[3]], writes=[gtB])

                    for i in range(NT):
                        par = tile_no[0] % 2
                        tile_no[0] += 1
                        OB = [3 + 2 * par, 4 + 2 * par]
                        units = []
                        ng = (i + 4) // 4
                        for c in range(2):
                            for g in range(ng):
                                for hh in range(2):
                                    units.append((c, g, hh))
                        pend = []

                        def qk_exp(u, idx):
                            c, g, hh = u
                            js = list(range(4 * g, min(4 * g + 4, i + 1)))
                            bk = idx % 3
                            rd = [qtB, ktB, xabB, cB]
                            kb.begin("pe", reads=rd, writes=[PSB[bk]])
                            for jj, j in enumerate(js):
                                near = j >= i - 1
                                mm = pe.matmul(PS[:, bk, jj * 128:(jj + 1) * 128],
                                               kt[:, c, hh, j * 128:(j + 1) * 128],
                                               qt[:, c, i * 128:(i + 1) * 128],
                                               start=True, stop=not near)
                                if near:
                                    case = 0 if j == i else 1
                                    mm = pe.matmul(PS[:, bk, jj * 128:(jj + 1) * 128], jmat[:, :],
                                                   XAB[:, 2 * hp + hh, case, :], start=False, stop=True)
                            kb.end("pe", mm, reads=rd, writes=[PSB[bk]])
                            pt, ptB = pt_r.next()
                            w = len(js) * 128
                            kb.op("act", lambda: act.activation(out=pt[:, 0:w], in_=PS[:, bk, 0:w], func=AF.Exp),
                                  reads=[PSB[bk]], writes=[ptB])
                            return (u, js, pt, ptB)

                        def pv(item):
                            (c, g, hh), js, pt, ptB = item
                            ob = OB[hh]
                            kb.begin("pe", reads=[ptB, vtB], writes=[PSB[ob]])
                            for jj, j in enumerate(js):
                                mm = pe.matmul(PS[:, ob, c * 129:(c + 1) * 129], pt[:, jj * 128:(jj + 1) * 128],
                                               vt[:, j, hh, :], start=(j == 0), stop=(j == i))
                            kb.end("pe", mm, reads=[ptB, vtB], writes=[PSB[ob]], partial=not (c == 0 and g == 0))

                        for idx, u in enumerate(units):
                            pend.append(qk_exp(u, idx))
                            if len(pend) > 2:
                                pv(pend.pop(0))
                        while pend:
                            pv(pend.pop(0))

                        tmps = []
                        for hh in range(2):
                            sc, scB = sc_r.next()
                            o1, o1B = o1_r.next()
                            o2, o2B = o2_r.next()
                            ot, otB = ot_r.next()
                            tmps.append((sc, scB, o1, o1B, o2, o2B, ot, otB, scB[0], scB[2], scB[3], scB[4], scB[5]))
                        for hh in range(2):
                            sc, scB, o1, o1B, o2, o2B, ot, otB, recB, nlB, ssB, vB, rB = tmps[hh]
                            ob = OB[hh]
                            den = PS[:, ob, 0:258].rearrange("p (c n) -> p c n", c=2)[:, :, 128]
                            kb.op("dve", lambda: dve.reciprocal(out=sc[:, 0:2], in_=den), reads=[PSB[ob]], writes=[recB, scB[1]])
                        for hh in range(2):
                            sc, scB, o1, o1B, o2, o2B, ot, otB, recB, nlB, ssB, vB, rB = tmps[hh]
                            kb.op("dve", lambda: dve.tensor_tensor(out=sc[:, 2:3], in0=sc[:, 1:2], in1=neg_lam[:], op=ALU.mult),
                                  reads=[recB, smallB], writes=[nlB])
                        for hh in range(2):
                            sc, scB, o1, o1B, o2, o2B, ot, otB, recB, nlB, ssB, vB, rB = tmps[hh]
                            ob = OB[hh]
                            kb.op("dve", lambda: dve.tensor_scalar(out=o1[:], in0=PS[:, ob, 0:128], scalar1=sc[:, 0:1], scalar2=None,
                                                                   op0=ALU.mult), reads=[PSB[ob], recB], writes=[o1B])
                        for hh in range(2):
                            sc, scB, o1, o1B, o2, o2B, ot, otB, recB, nlB, ssB, vB, rB = tmps[hh]
                            ob = OB[hh]
                            kb.op("dve", lambda: dve.scalar_tensor_tensor(out=o2[:], in0=PS[:, ob, 129:257], scalar=sc[:, 2:3],
                                                                         in1=o1[:], op0=ALU.mult, op1=ALU.add),
                                  reads=[PSB[ob], nlB, o1B], writes=[o2B])
                        for hh in range(2):
                            sc, scB, o1, o1B, o2, o2B, ot, otB, recB, nlB, ssB, vB, rB = tmps[hh]
                            kb.op("dve", lambda: dve.scalar_tensor_tensor(out=junk2[:], in0=o2[:], scalar=1.0, in1=o2[:],
                                                                         op0=ALU.mult, op1=ALU.mult, accum_out=sc[:, 3:4]),
                                  reads=[o2B], writes=[junk2B, ssB])
                        for hh in range(2):
                            sc, scB, o1, o1B, o2, o2B, ot, otB, recB, nlB, ssB, vB, rB = tmps[hh]
                            rstd_ops(sc[:, 3:4], ssB, sc[:, 5:6], rB, 1.0 / 128, sc[:, 4:5], vB)
                        for hh in range(2):
                            sc, scB, o1, o1B, o2, o2B, ot, otB, recB, nlB, ssB, vB, rB = tmps[hh]
                            kb.op("dve", lambda: dve.scalar_tensor_tensor(out=o1[:], in0=o2[:], scalar=sc[:, 5:6], in1=gsub_bc[:],
                                                                         op0=ALU.mult, op1=ALU.mult),
                                  reads=[o2B, rB, gsB], writes=[o1B])
                        for hh in range(2):
                            sc, scB, o1, o1B, o2, o2B, ot, otB, recB, nlB, ssB, vB, rB = tmps[hh]
                            kb.op("dve", lambda: dve.tensor_tensor(out=ot[:], in0=o1[:], in1=gt[:, i, hh * 128:(hh + 1) * 128],
                                                                   op=ALU.mult), reads=[o1B, gtB], writes=[otB])
                        def tr_closure(tm=tmps, hp_=hp, i_=i):
                            tbk = 7
                            kb.begin("pe", reads=[tm[0][7], tm[1][7], cB], writes=[PSB[tbk]])
                            for hh_ in range(2):
                                mm_ = pe.matmul(PS[:, tbk, hh_ * 128:(hh_ + 1) * 128], tm[hh_][6][:, :], ident[:, :],
                                                start=True, stop=True)
                            kb.end("pe", mm_, reads=[tm[0][7], tm[1][7], cB], writes=[PSB[tbk]])
                            kb.op("act", lambda: act.activation(out=oT[:, 2 * hp_:2 * hp_ + 2, i_ * 128:(i_ + 1) * 128],
                                                                in_=PS[:, tbk, 0:256].rearrange("p (c n) -> p c n", c=2),
                                                                func=AF.Copy),
                                  reads=[PSB[tbk]], writes=[oTB], partial=True)
                        defer2.append(tr_closure)
                        while len(defer2) > DEFER2:
                            defer2.pop(0)()
                while defer2:
                    defer2.pop(0)()
                kb.barrier()

            with ExitStack() as p3:
                VB = sb("VB", [128, NT, 8, 129], BF16, p3)
                VBB = kb.buf("VB")
                qbt_r = Ring(kb, p3, nc, "qbt", [128, 16, 128], BF16, 2, dma=True)
                iqt_r = Ring(kb, p3, nc, "iqt", [128, 8, 128], BF16, 2, dma=True)
                gbt_r = Ring(kb, p3, nc, "gbt", [128, 1024], BF16, 2, dma=True)
                diag_r = Ring(kb, p3, nc, "diag", [128, 16, 128], BF16, 1)
                rr_r = Ring(kb, p3, nc, "rr", [128, 512], BF16, 5)
                pt_r = Ring(kb, p3, nc, "pt3", [128, 512], BF16, 5)
                score = sb("score", [128, T], F32, p3)
                scoreB = kb.buf("score")
                nm_r = Ring(kb, p3, nc, "nm", [128, T], BF16, 3)
                bs_r = CRing(kb, p3, nc, "bs", 8, 3)
                zbuf = sb("zbuf", [128, T], F32, p3)
                zB = kb.buf("zbuf")
                z8_r = Ring(kb, p3, nc, "z8", [128, 24], F32, 2)
                ot_r = Ring(kb, p3, nc, "ot3", [128, 128], BF16, 12)
                rc_r = Ring(kb, p3, nc, "rc3", [128, 1], F32, 4)
                deferred = []

                def run_deferred(keep):
                    while len(deferred) > keep:
                        n = min(4, len(deferred))
                        while n > 1 and not (deferred[n - 1][3] == deferred[0][3] and deferred[n - 1][2] == deferred[0][2] + n - 1):
                            n -= 1
                        batch = [deferred.pop(0) for _ in range(n)]
                        rdl = [x_[1] for x_ in batch] + [cB]
                        kb.begin("pe", reads=rdl, writes=[PSB[TB_]])
                        for q, (ot_, otB_, h_, i_) in enumerate(batch):
                            mm_ = pe.matmul(PS[:, TB_, q * 128:(q + 1) * 128], ot_[:, :], ident[:, :], start=True, stop=True)
                        kb.end("pe", mm_, reads=rdl, writes=[PSB[TB_]])
                        h0, i0 = batch[0][2], batch[0][3]
                        kb.op("act", lambda: act.activation(out=oT[:, 8 + h0:8 + h0 + n, i0 * 128:(i0 + 1) * 128],
                                                            in_=PS[:, TB_, 0:n * 128].rearrange("p (c n) -> p c n", c=n),
                                                            func=AF.Copy),
                              reads=[PSB[TB_]], writes=[oTB], partial=True)

                kb.op("dve", lambda: dve.memset(VB[:, :, :, 128:129], 1.0), writes=[VBB], partial=True)
                for j in range(NT):
                    for half in range(2):
                        bk = nextbank(0, 4)
                        kb.begin("pe", reads=[ckvTB, wuvB], writes=[PSB[bk]])
                        for h4 in range(4):
                            h = half * 4 + h4
                            for cc in range(2):
                                mm = pe.matmul(PS[:, bk, h4 * 128:(h4 + 1) * 128], ckvT[:, cc, j * 128:(j + 1) * 128],
                                               wuv[:, h, cc, :], start=(cc == 0), stop=(cc == 1))
                        kb.end("pe", mm, reads=[ckvTB, wuvB], writes=[PSB[bk]])
                        kb.op("act", lambda: act.activation(out=VB[:, j, half * 4:(half + 1) * 4, 0:128],
                                                            in_=PS[:, bk, :].rearrange("p (h n) -> p h n", h=4), func=AF.Copy),
                              reads=[PSB[bk]], writes=[VBB], partial=True)

                XBK = [0, 1]
                SBK = [2, 3]
                OB = [4, 5]
                SCB = 6
                TB_ = 7
                xcnt = [0]
                scnt = [0]
                nm_of = {}

                def gen_index(i):
                    L = (i + 1) * 128
                    iqt, iqtB = iqt_r.next()
                    kb.dma("sp", iqt[:], IQT[b].rearrange("c p t -> p c t")[:, :, i * 128:(i + 1) * 128], iqtB.dsem,
                           reads=[scrB[6]], writes=[iqtB])
                    diag, diagB = diag_r.next()
                    id_ap = ident[:, :]
                    iw_ap = iw_sb[:, i, :]
                    id_bc = bass.AP(id_ap.tensor, id_ap.offset, [list(id_ap.ap[0]), [0, 16], [1, 128]])
                    iw_bc = bass.AP(iw_ap.tensor, iw_ap.offset, [list(iw_ap.ap[0]), [1, 16], [0, 128]])
                    kb.op("dve", lambda: dve.tensor_tensor(out=diag[:], in0=id_bc, in1=iw_bc, op=ALU.mult),
                          reads=[cB, iwB], writes=[diagB])
                    yield
                    nsc = (L + 511) // 512
                    for sc_i in range(nsc):
                        s0 = sc_i * 512
                        w = min(512, L - s0)
                        pend = []

                        def idx_x(h16):
                            m, hh = h16 // 2, h16 % 2
                            bk = xcnt[0] % 4
                            xcnt[0] += 1
                            kb.begin("pe", reads=[iqtB, ikTB], writes=[PSB[bk]])
                            mm = pe.matmul(PS[:, bk, 0:w], iqt[:, m, :], ikT2[:, hh, s0:s0 + w], start=True, stop=True)
                            kb.end("pe", mm, reads=[iqtB, ikTB], writes=[PSB[bk]])
                            rr, rrB = rr_r.next()
                            if h16 % 4 == 0:
                                kb.op("act", lambda: act.activation(out=rr[:, 0:w], in_=PS[:, bk, 0:w], func=AF.Relu),
                                      reads=[PSB[bk]], writes=[rrB])
                            else:
                                kb.op("dve", lambda: dve.tensor_scalar(out=rr[:, 0:w], in0=PS[:, bk, 0:w], scalar1=0.0, scalar2=None,
                                                                       op0=ALU.max), reads=[PSB[bk]], writes=[rrB])
                            return (h16, rr, rrB)

                        def idx_acc(item):
                            h16, rr, rrB = item
                            last = (h16 == 15)
                            has_diag = (s0 + w == L)
                            kb.begin("pe", reads=[rrB, diagB, cB], writes=[PSB[SCB]])
                            mm = pe.matmul(PS[:, SCB, 0:w], diag[:, h16, :], rr[:, 0:w], start=(h16 == 0),
                                           stop=(last and not has_diag))
                            if last and has_diag:
                                mm = pe.matmul(PS[:, SCB, w - 128:w], ident[:, :], negtri[:, :], start=False, stop=True)
                            kb.end("pe", mm, reads=[rrB, diagB, cB], writes=[PSB[SCB]], partial=(h16 > 0))

                        for h16 in range(16):
                            pend.append(idx_x(h16))
                            if len(pend) > 3:
                                idx_acc(pend.pop(0))
                                yield
                        while pend:
                            idx_acc(pend.pop(0))
                        kb.op("dve", lambda: dve.tensor_copy(out=score[:, s0:s0 + w], in_=PS[:, SCB, 0:w]),
                              reads=[PSB[SCB]], writes=[scoreB], partial=(sc_i > 0))
                        yield
                    if dbg:
                        kb.dma("sp", DBG_SC[b, i, :, 0:L], score[:, 0:L], dbgB.dsem, reads=[scoreB], writes=[dbgB], partial=True)
                    bs, bsB = bs_r.next()
                    cB_, mxB, cntB, tB3, eB, txB = bsB[0], bsB[1], bsB[2], bsB[3], bsB[4], bsB[5]
                    thrB = mxB
                    if L <= TOPK:
                        kb.op("dve", lambda: dve.memset(bs[:, 1:2], -1e29), writes=[thrB])
                    else:
                        kb.op("dve", lambda: dve.tensor_reduce(out=bs[:, 1:2], in_=score[:, 0:L], axis=AX.X, op=ALU.max),
                              reads=[scoreB], writes=[mxB])
                        wk = BIS_W / 2.0
                        kb.op("dve", lambda: dve.tensor_scalar(out=bs[:, 0:1], in0=bs[:, 1:2], scalar1=-1.0, scalar2=BIS_W - wk,
                                                               op0=ALU.mult, op1=ALU.add), reads=[mxB], writes=[cB_])
                        thrS = 2.0 * TOPK - L - 0.5
                        for it in range(NBIS):
                            kb.op("act", lambda: act.activation(out=zbuf[:, 0:L], in_=score[:, 0:L], func=AF.Sign,
                                                                bias=bs[:, 0:1], scale=1.0, accum_out=bs[:, 2:3]),
                                  reads=[scoreB, cB_], writes=[zB, cntB])
                            kb.op("act", lambda: act.activation(out=bs[:, 3:4], in_=bs[:, 2:3], func=AF.Sign, bias=-thrS, scale=1.0),
                                  reads=[cntB], writes=[tB3])
                            kb.op("act", lambda: act.activation(out=bs[:, 0:1], in_=bs[:, 3:4], func=AF.Identity, scale=-wk / 2.0,
                                                                bias=bs[:, 0:1]),
                                  reads=[tB3, cB_], writes=[cB_])
                            wlast = wk
                            wk = wk / 2.0
                            yield
                        for _ in range(4):
                            yield
                        nB = bsB[6]
                        kb.op("act", lambda: act.activation(out=bs[:, 6:7], in_=bs[:, 0:1], func=AF.Identity, scale=1.0,
                                                            bias=wlast / 2.0), reads=[cB_], writes=[nB])
                        kb.op("dve", lambda: dve.tensor_scalar(out=bs[:, 1:2], in0=bs[:, 6:7], scalar1=-1.0, scalar2=None,
                                                               op0=ALU.mult), reads=[nB], writes=[thrB])
                        kb.op("act", lambda: act.activation(out=zbuf[:, 0:L], in_=score[:, 0:L], func=AF.Sign,
                                                            bias=bs[:, 6:7], scale=1.0, accum_out=bs[:, 2:3]),
                              reads=[scoreB, nB], writes=[zB, cntB])
                        kb.op("dve", lambda: dve.tensor_scalar(out=bs[:, 4:5], in0=bs[:, 2:3], scalar1=0.5, scalar2=L / 2.0 - TOPK,
                                                               op0=ALU.mult, op1=ALU.add), reads=[cntB], writes=[eB])
                        kb.op("dve", lambda: dve.tensor_scalar(out=bs[:, 4:5], in0=bs[:, 4:5], scalar1=7.0, scalar2=0.0,
                                                               op0=ALU.min, op1=ALU.max), reads=[eB], writes=[eB])
                        kb.op("dve", lambda: dve.tensor_scalar(out=zbuf[:, 0:L], in0=score[:, 0:L], scalar1=bs[:, 1:2], scalar2=-1e36,
                                                               op0=ALU.is_lt, op1=ALU.mult), reads=[scoreB, thrB], writes=[zB])
                        kb.op("dve", lambda: dve.scalar_tensor_tensor(out=zbuf[:, 0:L], in0=score[:, 0:L], scalar=-1.0,
                                                                     in1=zbuf[:, 0:L], op0=ALU.mult, op1=ALU.add),
                              reads=[scoreB, zB], writes=[zB])
                        z8, z8B = z8_r.next()
                        z8a, z8b, z8c = kb.buf(), kb.buf(), kb.buf()
                        kb.op("dve", lambda: dve.max(out=z8[:, 0:8], in_=zbuf[:, 0:L]), reads=[zB], writes=[z8B])
                        kb.op("dve", lambda: dve.tensor_scalar(out=z8[:, 8:16], in0=iota8[:, :], scalar1=bs[:, 4:5], scalar2=None,
                                                               op0=ALU.is_equal), reads=[smallB, eB, z8B], writes=[z8B])
                        kb.op("dve", lambda: dve.scalar_tensor_tensor(out=z8[:, 16:24], in0=z8[:, 8:16], scalar=-1.0, in1=z8[:, 0:8],
                                                                     op0=ALU.mult, op1=ALU.mult, accum_out=bs[:, 5:6]),
                              reads=[z8B], writes=[z8B, txB])
                        thrB = txB
                        if dbg:
                            kb.dma("sp", DBG_BS[b, i, :, 0:6], bs[:, 0:6], dbgB.dsem, reads=[txB, eB, cntB, mxB, cB_], writes=[dbgB], partial=True)
                            kb.dma("sp", DBG_Z8[b, i, :, :], z8[:, :], dbgB.dsem, reads=[z8B], writes=[dbgB], partial=True)
                        yield
                    tcol = 1 if L <= TOPK else 5
                    if dbg:
                        kb.dma("sp", DBG_THR[b, i, :, :], bs[:, tcol:tcol + 1], dbgB.dsem, reads=[thrB], writes=[dbgB], partial=True)
                    nm, nmB = nm_r.next()
                    kb.op("dve", lambda: dve.tensor_scalar(out=nm[:, 0:L], in0=score[:, 0:L], scalar1=bs[:, tcol:tcol + 1], scalar2=NEG,
                                                           op0=ALU.is_lt, op1=ALU.mult),
                          reads=[scoreB, thrB], writes=[nmB])
                    nm_of[i] = (nm, nmB)
                    yield

                def gen_battn(i):
                    nm, nmB = nm_of.pop(i)
                    qbt, qbtB = qbt_r.next()
                    gbt, gbtB = gbt_r.next()
                    kb.dma("sp", qbt[:], QBT[b].rearrange("c p t -> p c t")[:, :, i * 128:(i + 1) * 128], qbtB.dsem,
                           reads=[scrB[4]], writes=[qbtB])
                    kb.dma("sp", gbt[:], GB[b, i * 128:(i + 1) * 128, :], gbtB.dsem, reads=[scrB[5]], writes=[gbtB])
                    ng = (i + 4) // 4
                    units = [(h, g) for h in range(8) for g in range(ng)]
                    pend = []

                    def qk_exp_b(u):
                        h, g = u
                        js = list(range(4 * g, min(4 * g + 4, i + 1)))
                        bk = xcnt[0] % 4
                        xcnt[0] += 1
                        rd = [qbtB, ckvTB, nmB, xabB, cB]
                        kb.begin("pe", reads=rd, writes=[PSB[bk]])
                        for jj, j in enumerate(js):
                            near = j >= i - 1
                            o_ap = PS[:, bk, jj * 128:(jj + 1) * 128]
                            for cc in range(2):
                                mm = pe.matmul(o_ap, ckvT[:, cc, j * 128:(j + 1) * 128], qbt[:, 2 * h + cc, :],
                                               start=(cc == 0), stop=False)
                            mm = pe.matmul(o_ap, nm[:, j * 128:(j + 1) * 128], ident[:, :], start=False, stop=not near)
                            if near:
                                case = 0 if j == i else 1
                                mm = pe.matmul(o_ap, jmat[:, :], XAB[:, 8 + h, case, :], start=False, stop=True)
                        kb.end("pe", mm, reads=rd, writes=[PSB[bk]])
                        pt, ptB = pt_r.next()
                        w = len(js) * 128
                        kb.op("act", lambda: act.activation(out=pt[:, 0:w], in_=PS[:, bk, 0:w], func=AF.Exp),
                              reads=[PSB[bk]], writes=[ptB])
                        return (u, js, pt, ptB)

                    def pv_b(item):
                        (h, g), js, pt, ptB = item
                        ob = OB[h % 2]
                        kb.begin("pe", reads=[ptB, VBB], writes=[PSB[ob]])
                        for jj, j in enumerate(js):
                            mm = pe.matmul(PS[:, ob, 0:129], pt[:, jj * 128:(jj + 1) * 128], VB[:, j, h, :],
                                           start=(j == 0), stop=(j == i))
                        kb.end("pe", mm, reads=[ptB, VBB], writes=[PSB[ob]], partial=(g > 0))
                        if g == ng - 1:
                            rc, rcB = rc_r.next()
                            ot, otB = ot_r.next()
                            kb.op("dve", lambda: dve.reciprocal(out=rc[:], in_=PS[:, ob, 128:129]), reads=[PSB[ob]], writes=[rcB])
                            kb.op("dve", lambda: dve.scalar_tensor_tensor(out=ot[:], in0=PS[:, ob, 0:128], scalar=rc[:, 0:1],
                                                                         in1=gbt[:, h * 128:(h + 1) * 128], op0=ALU.mult,
                                                                         op1=ALU.mult),
                                  reads=[PSB[ob], rcB, gbtB], writes=[otB])
                            deferred.append((ot, otB, h, i))
                            run_deferred(DEFER3) if len(deferred) >= DEFER3 + 4 else None

                    for u in units:
                        pend.append(qk_exp_b(u))
                        if len(pend) > 3:
                            pv_b(pend.pop(0))
                            yield
                    while pend:
                        pv_b(pend.pop(0))
                        yield

                def n_steps_index(i):
                    L = (i + 1) * 128
                    nsc = (L + 511) // 512
                    return 1 + nsc * 16 + (0 if L <= TOPK else NBIS + 5) + 1

                def n_steps_battn(i):
                    return 8 * ((i + 4) // 4)

                LOOK = 2
                for i0 in range(min(LOOK, NT)):
                    for _ in gen_index(i0):
                        pass
                for i in range(NT):
                    ga = gen_battn(i)
                    na = n_steps_battn(i)
                    gi = gen_index(i + LOOK) if i + LOOK < NT else None
                    ni = n_steps_index(i + LOOK) if gi is not None else 0
                    da = di = 0
                    a_alive, i_alive = True, gi is not None
                    while a_alive or i_alive:
                        pick_a = a_alive and (not i_alive or da * ni <= di * na)
                        if pick_a:
                            try:
                                next(ga)
                                da += 1
                            except StopIteration:
                                a_alive = False
                        else:
                            try:
                                next(gi)
                                di += 1
                            except StopIteration:
                                i_alive = False
                run_deferred(0)
                if dbg:
                    kb.dma("sp", DBG_OT[b], oT[:], dbgB.dsem, reads=[oTB], writes=[dbgB], partial=True)
                kb.barrier()

            with ExitStack() as p4:
                kb.dma("sp", g_bc[:], bcast(gpost_d[0:1, :], D), gB.dsem, writes=[gB])
                wo = sb("wo", [128, 16, D], BF16, p4)
                woB = kb.buf("wo", dma=True)
                for q in range(4):
                    kb.dma("pool", wo[:, q * 4:(q + 1) * 4, :],
                           wout_d[q * 512:(q + 1) * 512, :].rearrange("(k p) n -> p k n", p=128),
                           woB.dsem, writes=[woB], partial=True)
                xt_r = Ring(kb, p4, nc, "xt4", [128, D], F32, 2, dma=True)
                yn_r = Ring(kb, p4, nc, "yn4", [128, D], F32, 2, dma=True)
                st_r = CRing(kb, p4, nc, "st4", 4, 2)
                for i in range(NT):
                    xt, xtB = xt_r.next()
                    kb.dma("sp", xt[:], x_d[b, i * 128:(i + 1) * 128, :], xtB.dsem, writes=[xtB])
                    g4 = (i % 2) * 4
                    for n4 in range(4):
                        bk = g4 + n4
                        kb.begin("pe", reads=[oTB, woB], writes=[PSB[bk]])
                        for k in range(16):
                            mm = pe.matmul(PS[:, bk, :], oT[:, k, i * 128:(i + 1) * 128], wo[:, k, n4 * 512:(n4 + 1) * 512],
                                           start=(k == 0), stop=(k == 15))
                        kb.end("pe", mm, reads=[oTB, woB], writes=[PSB[bk]])
                    ybanks = [PSB[g4 + n] for n in range(4)]
                    yap = PS[:, g4:g4 + 4, :]
                    st, sB_ = st_r.next()
                    ssB, vB, rB = sB_[0], sB_[1], sB_[2]
                    yn, ynB = yn_r.next()
                    kb.op("act", lambda: act.activation(out=yn[:].rearrange("p (a n) -> p a n", a=4), in_=yap, func=AF.Square,
                                                        accum_out=st[:, 0:1]),
                          reads=ybanks, writes=[ynB, ssB])
                    rstd_ops(st[:, 0:1], ssB, st[:, 2:3], rB, 1.0 / D, st[:, 1:2], vB)
                    kb.op("dve", lambda: dve.scalar_tensor_tensor(out=yn[:].rearrange("p (a n) -> p a n", a=4), in0=yap,
                                                                 scalar=st[:, 2:3],
                                                                 in1=g_bc[:].rearrange("p (a n) -> p a n", a=4),
                                                                 op0=ALU.mult, op1=ALU.mult),
                          reads=ybanks + [rB, gB], writes=[ynB])
                    kb.op("dve", lambda: dve.tensor_tensor(out=yn[:], in0=yn[:], in1=xt[:], op=ALU.add),
                          reads=[ynB, xtB], writes=[ynB])
                    kb.dma("sp", out_d[b, i * 128:(i + 1) * 128, :], yn[:], ynB.dsem, reads=[ynB], writes=[])
                kb.barrier()
            p234.close()
        print("build: waits=%d counts=%s" % (kb.nwaits, {k: v for k, v in kb.cnt.items() if k in kb.E}))
    return nc


_NC_CACHE = {}


def _get_nc(T, NSEQ, dbg=False):
    key = (T, NSEQ, dbg)
    if key not in _NC_CACHE:
        _NC_CACHE[key] = build_nc(T, NSEQ, dbg)
    return _NC_CACHE[key]


def make_in_maps(inputs, n_cores, nseq):
    c = _consts()
    f = lambda a: np.ascontiguousarray(np.asarray(a, dtype=np.float32))
    shared = {
        "w_in": f(inputs["w_in"])[0],
        "w_out": f(inputs["w_out"])[0],
        "norm_pre_g": f(inputs["norm_pre_g"]),
        "norm_post_g": f(inputs["norm_post_g"]),
        "lambda_q1": f(inputs["lambda_q1"]),
        "lambda_k1": f(inputs["lambda_k1"]),
        "lambda_q2": f(inputs["lambda_q2"]),
        "lambda_k2": f(inputs["lambda_k2"]),
        "subln_g": f(inputs["subln_g"]),
        "kv_norm_g": f(inputs["kv_norm_g"]),
        "idx_k_norm_g": f(inputs["idx_k_norm_g"]),
        "w_uv": f(inputs["w_uv"])[0],
        "rel_bias": f(inputs["rel_bias"]),
    }
    shared.update(c)
    x = f(inputs["x"])
    maps = []
    for i in range(n_cores):
        m = dict(shared)
        m["x"] = np.ascontiguousarray(x[i * nseq:(i + 1) * nseq])
        maps.append(m)
    return maps


def kernel(**inputs):
    x = np.asarray(inputs["x"])
    B, T, _ = x.shape
    n_cores = 8
    nseq = B // n_cores
    nc = _get_nc(T, nseq)
    in_maps = make_in_maps(inputs, n_cores, nseq)
    res = run_bass_kernel_spmd(nc, in_maps, core_ids=list(range(n_cores)))
    out = np.concatenate([np.asarray(r["out"]) for r in res.results], axis=0)
    return out.astype(np.float32)
```
